# Optimizing a Trainium2 kernel written in Bass

```python
import math
import jax, jax.numpy as jnp
from jax import lax
import numpy as np

D_MODEL = 2048
BATCH = 4
SEQ = 4096
DEPTH = 2

MEM_TOKENS = 256
EPS = 1e-6
ROPE_THETA = 500000.0
N_EVEN = (DEPTH + 1) // 2
N_ODD = DEPTH // 2

ML_HEADS = 4
ML_DV = D_MODEL // 2 // ML_HEADS
ML_DK = ML_DV // 2
ML_CHUNK = 64
ML_CONV = 4
ML_FORGET_BIAS = 3.0
RW_HEAD = 64
RW_HEADS = D_MODEL // 2 // RW_HEAD
RW_DIM = RW_HEADS * RW_HEAD
RW_LORA_W = 64
RW_LORA_A = 64
RW_LORA_G = 128
RW_LN_EPS = 64e-5
ML_SPLITS = (2 * ML_HEADS * ML_DK, ML_HEADS * ML_DV, ML_HEADS * ML_DV, 2 * ML_HEADS)
RW_SPLITS = (RW_DIM, RW_DIM, RW_DIM, RW_LORA_W, RW_LORA_A, RW_LORA_G)
ML_COLS = sum(ML_SPLITS)
RW_COLS = sum(RW_SPLITS)
AB_COLS = ML_COLS + RW_COLS

NSA_HEADS = 16
NSA_KV = 4
NSA_REP = NSA_HEADS // NSA_KV
NSA_HD = D_MODEL // NSA_HEADS
ROPE_DIM = NSA_HD // 4
CMP_BLOCK = 32
CMP_STRIDE = 16
SEL_BLOCK = 64
SEL_TOPK = 16
WINDOW = 512
NSA_QCHUNK = 32
NSA_SPLITS = (NSA_HEADS * NSA_HD,) + (NSA_KV * NSA_HD,) * 6 + (3 * NSA_HEADS,)
NSA_COLS = sum(NSA_SPLITS)

CA_HEADS = 4
CA_HD = D_MODEL // CA_HEADS

MOE_GROUPS = 8
MOE_PER_GROUP = 8
MOE_EXPERTS = MOE_GROUPS * MOE_PER_GROUP
MOE_TOPK = 2
MOE_FF = D_MODEL // 4
MOE_ROWS = 128

kernel_name = 'hybrid_mlstm_rwkv7_nsa_hmoe'

F32 = jnp.float32


def _split(x, sizes):
    return jnp.split(x, [int(s) for s in np.cumsum(sizes)[:-1]], axis=-1)


def _rmsnorm(x, w):
    xf = x.astype(F32)
    y = xf * lax.rsqrt(jnp.mean(xf * xf, axis=-1, keepdims=True) + EPS)
    return (y * w.astype(F32)).astype(x.dtype)


def _masked_softmax(s, mask):
    s = jnp.where(mask, s.astype(F32), -jnp.inf)
    m = jnp.max(s, axis=-1, keepdims=True)
    m = jnp.where(jnp.isfinite(m), m, 0.0)
    e = jnp.exp(s - m)
    d = jnp.sum(e, axis=-1, keepdims=True)
    return e / jnp.where(d > 0, d, 1.0)


def _rope(x, pos):
    half = ROPE_DIM // 2
    inv = 1.0 / (ROPE_THETA ** (jnp.arange(half, dtype=F32) / half))
    ang = pos.astype(F32)[:, None] * inv[None, :]
    shape = (ang.shape[0],) + (1,) * (x.ndim - 3) + (half,)
    cos = jnp.cos(ang).reshape(shape)
    sin = jnp.sin(ang).reshape(shape)
    xf = x.astype(F32)
    x1 = xf[..., :half]
    x2 = xf[..., half:ROPE_DIM]
    out = jnp.concatenate([x1 * cos - x2 * sin, x2 * cos + x1 * sin, xf[..., ROPE_DIM:]], axis=-1)
    return out.astype(x.dtype)


def _token_shift(p):
    return jnp.pad(p, ((0, 0), (1, 0), (0, 0)))[:, :-1]


def _causal_conv(x, w, b):
    k_len = w.shape[0]
    s_len = x.shape[1]
    xp = jnp.pad(x, ((0, 0), (k_len - 1, 0), (0, 0)))
    y = b
    for j in range(k_len):
        y = y + xp[:, j:j + s_len] * w[j]
    return y


def _mlstm(q, k, v, i_pre, f_pre):
    B, S, H, DK = q.shape
    DV = v.shape[-1]
    L = ML_CHUNK
    NC = S // L

    def chunks(t):
        t = t.astype(F32).reshape((B, NC, L, H) + t.shape[3:])
        return t.transpose((1, 0, 3, 2) + tuple(range(4, t.ndim)))

    qc = chunks(q) * (DK ** -0.5)
    kc = chunks(k)
    vc = chunks(v)
    ic = chunks(i_pre)
    lfc = chunks(jax.nn.log_sigmoid(f_pre.astype(F32)))
    causal = jnp.asarray(np.tril(np.ones((L, L), dtype=bool)))

    def step(carry, xs):
        C, n, m = carry
        q_, k_, v_, i_, lf = xs
        b = jnp.cumsum(lf, axis=-1)
        dmat = jnp.where(causal, b[..., :, None] - b[..., None, :] + i_[..., None, :], -jnp.inf)
        inter = b + m[..., None]
        m_row = jnp.maximum(inter, jnp.max(dmat, axis=-1))
        w_in = jnp.exp(dmat - m_row[..., None])
        w_st = jnp.exp(inter - m_row)
        s = jnp.einsum('bhjd,bhld->bhjl', q_, k_) * w_in
        num = w_st[..., None] * jnp.einsum('bhjd,bhde->bhje', q_, C) + jnp.einsum('bhjl,bhle->bhje', s, v_)
        den = w_st * jnp.einsum('bhjd,bhd->bhj', q_, n) + jnp.sum(s, axis=-1)
        h = num / jnp.maximum(jnp.abs(den), jnp.exp(-m_row))[..., None]
        b_last = b[..., -1]
        g_key = b_last[..., None] - b + i_
        m_new = jnp.maximum(b_last + m, jnp.max(g_key, axis=-1))
        wk = jnp.exp(g_key - m_new[..., None])
        decay = jnp.exp(b_last + m - m_new)
        C_new = decay[..., None, None] * C + jnp.einsum('bhld,bhle->bhde', k_ * wk[..., None], v_)
        n_new = decay[..., None] * n + jnp.einsum('bhl,bhld->bhd', wk, k_)
        return (C_new, n_new, m_new), h

    init = (jnp.zeros((B, H, DK, DV), F32), jnp.zeros((B, H, DK), F32), jnp.zeros((B, H), F32))
    _, hs = lax.scan(step, init, (qc, kc, vc, ic, lfc))
    return hs.transpose(1, 0, 3, 2, 4).reshape(B, S, H, DV)


def _rwkv7_scan(r, w, k, v, kk, a):
    def step(state, xs):
        r_t, w_t, k_t, v_t, kk_t, a_t = xs
        sk = jnp.einsum('bhvk,bhk->bhv', state, kk_t)
        state = (state * w_t[:, :, None, :] - sk[..., None] * (kk_t * a_t)[:, :, None, :]
                 + v_t[..., None] * k_t[:, :, None, :])
        return state, jnp.einsum('bhvk,bhk->bhv', state, r_t)

    B, S, H, N = r.shape
    xs = tuple(t.transpose(1, 0, 2, 3) for t in (r, w, k, v, kk, a))
    _, y = lax.scan(step, jnp.zeros((B, H, N, N), F32), xs)
    return y.transpose(1, 0, 2, 3)


def _ab_mixer(h, w_in, conv_w, conv_b, gate_b, mu, w0, w_up, a0, a_up, g_up, k_k, k_a, r_k, ln_w, ln_b, w_out):
    B, S, _ = h.shape
    p = h @ w_in
    ml_p, rw_p = p[..., :ML_COLS], p[..., ML_COLS:]
    qk, v_m, o_m, gif = _split(ml_p, ML_SPLITS)
    qk = jax.nn.silu(_causal_conv(qk, conv_w, conv_b))
    q_m, k_m = jnp.split(qk, 2, axis=-1)
    gif = (gif + gate_b).astype(F32)
    h_m = _mlstm(q_m.reshape(B, S, ML_HEADS, ML_DK), k_m.reshape(B, S, ML_HEADS, ML_DK),
                 v_m.reshape(B, S, ML_HEADS, ML_DV), gif[..., :ML_HEADS], gif[..., ML_HEADS:])
    y_m = jax.nn.sigmoid(o_m.astype(F32)) * h_m.reshape(B, S, ML_HEADS * ML_DV)
    rw_p = (rw_p + (_token_shift(rw_p) - rw_p) * mu).astype(F32)
    r, k, v, xw, xa, xg = _split(rw_p, RW_SPLITS)
    logw = -math.exp(-0.5) * jax.nn.sigmoid(w0 + jnp.tanh(xw) @ w_up)
    a = jax.nn.sigmoid(a0 + xa @ a_up)
    g = jax.nn.sigmoid(xg) @ g_up

    def heads(t):
        return t.reshape(B, S, RW_HEADS, RW_HEAD)

    kk = heads(k * k_k)
    kk = kk * lax.rsqrt(jnp.sum(kk * kk, axis=-1, keepdims=True) + 1e-12)
    k = k * (1.0 + (a - 1.0) * k_a)
    rh, kh, vh, ah = heads(r), heads(k), heads(v), heads(a)
    y = _rwkv7_scan(rh, jnp.exp(heads(logw)), kh, vh, kk, ah)
    y_mu = jnp.mean(y, axis=-1, keepdims=True)
    y_var = jnp.mean(jnp.square(y - y_mu), axis=-1, keepdims=True)
    yn = ((y - y_mu) * lax.rsqrt(y_var + RW_LN_EPS)).reshape(B, S, RW_DIM) * ln_w + ln_b
    bonus = (jnp.sum(rh * kh * r_k.reshape(RW_HEADS, RW_HEAD), axis=-1, keepdims=True) * vh).reshape(B, S, RW_DIM)
    y_r = (yn + bonus) * g
    y_cat = jnp.concatenate([y_m, y_r], axis=-1).astype(h.dtype)
    return y_cat @ w_out


def _nsa(h, w_in, gate_b, cmp_pos, cmp_w1, cmp_w2, w_out):
    B, S, _ = h.shape
    G, R, HD = NSA_KV, NSA_REP, NSA_HD
    QC = NSA_QCHUNK
    scale = HD ** -0.5
    pos = jnp.arange(S)
    q, kc, vc, ks, vs, kw, vw, gl = _split(h @ w_in, NSA_SPLITS)
    q = q.reshape(B, S, G, R, HD)

    def kvh(t):
        return t.reshape(B, S, G, HD)

    q_rot = _rope(q, pos)
    ks = _rope(kvh(ks), pos)
    kw = _rope(kvh(kw), pos)
    vs = kvh(vs)
    vw = kvh(vw)
    gates = jax.nn.sigmoid((gl + gate_b).astype(F32)).reshape(B, S, 3, G, R)

    n_cmp = (S - CMP_BLOCK) // CMP_STRIDE + 1
    cidx = np.arange(n_cmp)[:, None] * CMP_STRIDE + np.arange(CMP_BLOCK)[None, :]

    def compress(t, pe, w1, w2):
        blocks = t[:, cidx] + pe[None, None, :, None, :]
        return jax.nn.gelu(jnp.einsum('bjpgd,pde->bjge', blocks, w1)) @ w2

    k_cmp = compress(kvh(kc), cmp_pos[0], cmp_w1[0], cmp_w2[0])
    v_cmp = compress(kvh(vc), cmp_pos[1], cmp_w1[1], cmp_w2[1])
    cmp_end = jnp.asarray(cidx[:, -1])

    n_sel = S // SEL_BLOCK
    c0 = np.arange(n_cmp)[:, None] * CMP_STRIDE
    s0 = np.arange(n_sel)[None, :] * SEL_BLOCK
    ov = np.clip(np.minimum(c0 + CMP_BLOCK, s0 + SEL_BLOCK) - np.maximum(c0, s0), 0, None) / CMP_BLOCK
    ov = jnp.asarray(ov, dtype=F32)
    n_top = min(SEL_TOPK, n_sel)
    k_blk = ks.reshape(B, n_sel, SEL_BLOCK, G, HD).transpose(0, 3, 1, 2, 4)
    v_blk = vs.reshape(B, n_sel, SEL_BLOCK, G, HD).transpose(0, 3, 1, 2, 4)
    kw_pad = jnp.pad(kw, ((0, 0), (WINDOW, 0), (0, 0), (0, 0)))
    vw_pad = jnp.pad(vw, ((0, 0), (WINDOW, 0), (0, 0), (0, 0)))
    bi = jnp.arange(B)[:, None, None, None]
    gi = jnp.arange(G)[None, :, None, None]
    blk_ids = jnp.arange(n_sel)

    def chunk(c):
        t0 = c * QC
        qt = t0 + jnp.arange(QC)
        q_c = lax.dynamic_slice_in_dim(q, t0, QC, axis=1)
        qr_c = lax.dynamic_slice_in_dim(q_rot, t0, QC, axis=1)
        g_c = lax.dynamic_slice_in_dim(gates, t0, QC, axis=1)
        p_cmp = _masked_softmax(jnp.einsum('bqgrd,bjgd->bgrqj', q_c, k_cmp) * scale,
                                cmp_end[None, :] <= qt[:, None])
        o_cmp = jnp.einsum('bgrqj,bjgd->bqgrd', p_cmp.astype(v_cmp.dtype), v_cmp)
        imp = jnp.einsum('bgrqj,js->bgqs', p_cmp, ov)
        cur = (qt // SEL_BLOCK)[:, None]
        forced = (blk_ids[None, :] == 0) | (blk_ids[None, :] == cur) | (blk_ids[None, :] == cur - 1)
        score = jnp.where(forced, jnp.inf, jnp.where(blk_ids[None, :] <= cur, imp, -jnp.inf))
        top_v, top_i = lax.top_k(score, n_top)
        k_g = k_blk[bi, gi, top_i].reshape(B, G, QC, n_top * SEL_BLOCK, HD)
        v_g = v_blk[bi, gi, top_i].reshape(B, G, QC, n_top * SEL_BLOCK, HD)
        kpos = (top_i[..., None] * SEL_BLOCK + jnp.arange(SEL_BLOCK)).reshape(B, G, QC, n_top * SEL_BLOCK)
        kmask = jnp.repeat(top_v > -jnp.inf, SEL_BLOCK, axis=-1) & (kpos <= qt[None, None, :, None])
        p_slc = _masked_softmax(jnp.einsum('bqgrd,bgqkd->bgrqk', qr_c, k_g) * scale, kmask[:, :, None])
        o_slc = jnp.einsum('bgrqk,bgqkd->bqgrd', p_slc.astype(v_g.dtype), v_g)
        k_w = lax.dynamic_slice_in_dim(kw_pad, t0, QC + WINDOW, axis=1)
        v_w = lax.dynamic_slice_in_dim(vw_pad, t0, QC + WINDOW, axis=1)
        wpos = t0 - WINDOW + jnp.arange(QC + WINDOW)
        dist = qt[:, None] - wpos[None, :]
        wmask = (dist >= 0) & (dist < WINDOW) & (wpos[None, :] >= 0)
        p_win = _masked_softmax(jnp.einsum('bqgrd,bkgd->bgrqk', qr_c, k_w) * scale, wmask)
        o_win = jnp.einsum('bgrqk,bkgd->bqgrd', p_win.astype(v_w.dtype), v_w)
        return (g_c[:, :, 0, :, :, None] * o_cmp + g_c[:, :, 1, :, :, None] * o_slc
                + g_c[:, :, 2, :, :, None] * o_win)

    o = lax.map(chunk, jnp.arange(S // QC))
    o = o.transpose(1, 0, 2, 3, 4, 5).reshape(B, S, NSA_HEADS * HD).astype(h.dtype)
    return o @ w_out


def _cross_attn(h, m, wq, wk, wv, wo):
    B, S, D = h.shape
    M = m.shape[1]
    q = (h @ wq).reshape(B, S, CA_HEADS, CA_HD)
    k = (m @ wk).reshape(B, M, CA_HEADS, CA_HD)
    v = (m @ wv).reshape(B, M, CA_HEADS, CA_HD)
    s = jnp.einsum('bshd,bmhd->bhsm', q, k).astype(F32) * (CA_HD ** -0.5)
    p = jax.nn.softmax(s, axis=-1).astype(v.dtype)
    o = jnp.einsum('bhsm,bmhd->bshd', p, v).reshape(B, S, D)
    return o @ wo


def _hier_moe(h, wg, bg, we, be, w_gate, w_up, w_down):
    B, S, D = h.shape
    T = B * S
    A = T * MOE_TOPK
    xt = h.reshape(T, D)
    lg = (xt @ wg + bg).astype(F32)
    grp = jnp.argmax(lg, axis=-1)
    p_grp = jnp.take_along_axis(jax.nn.softmax(lg, axis=-1), grp[:, None], axis=-1)
    le = (xt @ we + be).astype(F32).reshape(T, MOE_GROUPS, MOE_PER_GROUP)
    le = jnp.take_along_axis(le, grp[:, None, None], axis=1)[:, 0]
    top_l, top_e = lax.top_k(le, MOE_TOPK)
    wts = (p_grp * jax.nn.softmax(top_l, axis=-1)).reshape(A)
    eid = (grp[:, None] * MOE_PER_GROUP + top_e).reshape(A)
    tok = jnp.repeat(jnp.arange(T, dtype=jnp.int32), MOE_TOPK)
    order = jnp.argsort(eid)
    e_s, tok_s, w_s = eid[order], tok[order], wts[order]
    counts = jax.ops.segment_sum(jnp.ones((A,), jnp.int32), eid, num_segments=MOE_EXPERTS)
    padded = (counts + MOE_ROWS - 1) // MOE_ROWS * MOE_ROWS
    ends = jnp.cumsum(padded)
    dest = (ends - padded)[e_s] + jnp.arange(A, dtype=jnp.int32) - (jnp.cumsum(counts) - counts)[e_s]
    n_chunks = -(-A // MOE_ROWS) + MOE_EXPERTS
    rows = n_chunks * MOE_ROWS
    row_tok = jnp.full((rows,), T, jnp.int32).at[dest].set(tok_s)
    row_w = jnp.zeros((rows,), F32).at[dest].set(w_s)
    chunk_e = jnp.minimum(jnp.searchsorted(ends, jnp.arange(n_chunks, dtype=jnp.int32) * MOE_ROWS, side='right'),
                          MOE_EXPERTS - 1)
    x_pad = jnp.concatenate([xt, jnp.zeros((1, D), xt.dtype)], axis=0)

    def expert_rows(args):
        t_idx, w_r, e = args
        xr = x_pad[t_idx]
        hid = jax.nn.silu(xr @ w_gate[e]) * (xr @ w_up[e])
        y = hid @ w_down[e]
        return y * w_r[:, None].astype(y.dtype)

    out = lax.map(expert_rows, (row_tok.reshape(n_chunks, MOE_ROWS), row_w.reshape(n_chunks, MOE_ROWS), chunk_e))
    y = jax.ops.segment_sum(out.reshape(rows, D), row_tok, num_segments=T + 1)[:T]
    return y.reshape(B, S, D).astype(h.dtype)


def setup_inputs(seed: int = 0) -> dict:
    key = jax.random.key(seed)
    ks = iter(jax.random.split(key, 48))
    D = D_MODEL

    def nrm(shape, scale):
        return scale * jax.random.normal(next(ks), shape, F32)

    def gain(shape):
        return 1.0 + nrm(shape, 0.05)

    gate_offset = jnp.concatenate([jnp.zeros((ML_HEADS,), F32), jnp.full((ML_HEADS,), ML_FORGET_BIAS, F32)])
    return {
        'x': nrm((BATCH, SEQ, D), 1.0),
        'mem': nrm((BATCH, MEM_TOKENS, D), 1.0),
        'norm_mix': gain((DEPTH, D)),
        'norm_cross': gain((DEPTH, D)),
        'norm_mem': gain((DEPTH, D)),
        'norm_ffn': gain((DEPTH, D)),
        'norm_final': gain((D,)),
        'ab_w_in': nrm((N_EVEN, D, AB_COLS), D ** -0.5),
        'ml_conv_w': nrm((N_EVEN, ML_CONV, 2 * ML_HEADS * ML_DK), 0.5),
        'ml_conv_b': nrm((N_EVEN, 2 * ML_HEADS * ML_DK), 0.02),
        'ml_gate_b': nrm((N_EVEN, 2 * ML_HEADS), 0.1) + gate_offset,
        'rw_mu': 0.5 + nrm((N_EVEN, RW_COLS), 0.15),
        'rw_w0': nrm((N_EVEN, RW_DIM), 0.5),
        'rw_w_up': nrm((N_EVEN, RW_LORA_W, RW_DIM), 0.1),
        'rw_a0': nrm((N_EVEN, RW_DIM), 0.1),
        'rw_a_up': nrm((N_EVEN, RW_LORA_A, RW_DIM), 0.5 * RW_LORA_A ** -0.5),
        'rw_g_up': nrm((N_EVEN, RW_LORA_G, RW_DIM), RW_LORA_G ** -0.5),
        'rw_k_k': 0.85 + nrm((N_EVEN, RW_DIM), 0.05),
        'rw_k_a': 1.0 + nrm((N_EVEN, RW_DIM), 0.05),
        'rw_r_k': nrm((N_EVEN, RW_DIM), 0.1),
        'rw_ln_w': gain((N_EVEN, RW_DIM)),
        'rw_ln_b': nrm((N_EVEN, RW_DIM), 0.02),
        'ab_w_out': nrm((N_EVEN, ML_HEADS * ML_DV + RW_DIM, D), (ML_HEADS * ML_DV + RW_DIM) ** -0.5),
        'nsa_w_in': nrm((N_ODD, D, NSA_COLS), D ** -0.5),
        'nsa_gate_b': nrm((N_ODD, 3 * NSA_HEADS), 0.1),
        'cmp_pos': nrm((N_ODD, 2, CMP_BLOCK, NSA_HD), 0.02),
        'cmp_w1': nrm((N_ODD, 2, CMP_BLOCK, NSA_HD, NSA_HD), (CMP_BLOCK * NSA_HD) ** -0.5),
        'cmp_w2': nrm((N_ODD, 2, NSA_HD, NSA_HD), NSA_HD ** -0.5),
        'nsa_w_out': nrm((N_ODD, NSA_HEADS * NSA_HD, D), (NSA_HEADS * NSA_HD) ** -0.5),
        'ca_wq': nrm((DEPTH, D, D), D ** -0.5),
        'ca_wk': nrm((DEPTH, D, D), D ** -0.5),
        'ca_wv': nrm((DEPTH, D, D), D ** -0.5),
        'ca_wo': nrm((DEPTH, D, D), D ** -0.5),
        'moe_wg': nrm((DEPTH, D, MOE_GROUPS), D ** -0.5),
        'moe_bg': nrm((DEPTH, MOE_GROUPS), 0.01),
        'moe_we': nrm((DEPTH, D, MOE_EXPERTS), D ** -0.5),
        'moe_be': nrm((DEPTH, MOE_EXPERTS), 0.01),
        'moe_w_gate': nrm((DEPTH, MOE_EXPERTS, D, MOE_FF), D ** -0.5),
        'moe_w_up': nrm((DEPTH, MOE_EXPERTS, D, MOE_FF), D ** -0.5),
        'moe_w_down': nrm((DEPTH, MOE_EXPERTS, MOE_FF, D), MOE_FF ** -0.5),
    }


def reference(x, mem, norm_mix, norm_cross, norm_mem, norm_ffn, norm_final,
              ab_w_in, ml_conv_w, ml_conv_b, ml_gate_b, rw_mu, rw_w0, rw_w_up, rw_a0, rw_a_up, rw_g_up,
              rw_k_k, rw_k_a, rw_r_k, rw_ln_w, rw_ln_b, ab_w_out,
              nsa_w_in, nsa_gate_b, cmp_pos, cmp_w1, cmp_w2, nsa_w_out,
              ca_wq, ca_wk, ca_wv, ca_wo,
              moe_wg, moe_bg, moe_we, moe_be, moe_w_gate, moe_w_up, moe_w_down):
    for l in range(DEPTH):
        j = l // 2
        hn = _rmsnorm(x, norm_mix[l])
        if l % 2 == 0:
            x = x + _ab_mixer(hn, ab_w_in[j], ml_conv_w[j], ml_conv_b[j], ml_gate_b[j], rw_mu[j], rw_w0[j],
                              rw_w_up[j], rw_a0[j], rw_a_up[j], rw_g_up[j], rw_k_k[j], rw_k_a[j], rw_r_k[j],
                              rw_ln_w[j], rw_ln_b[j], ab_w_out[j])
        else:
            x = x + _nsa(hn, nsa_w_in[j], nsa_gate_b[j], cmp_pos[j], cmp_w1[j], cmp_w2[j], nsa_w_out[j])
        x = x + _cross_attn(_rmsnorm(x, norm_cross[l]), _rmsnorm(mem, norm_mem[l]),
                            ca_wq[l], ca_wk[l], ca_wv[l], ca_wo[l])
        x = x + _hier_moe(_rmsnorm(x, norm_ffn[l]), moe_wg[l], moe_bg[l], moe_we[l], moe_be[l],
                          moe_w_gate[l], moe_w_up[l], moe_w_down[l])
    return _rmsnorm(x, norm_final)
```

```python
import math
import numpy as np
import concourse.bass as bass
import concourse.mybir as mybir
from concourse.alu_op_type import AluOpType as ALU
from concourse.bass_utils import run_bass_kernel_spmd

AF = mybir.ActivationFunctionType
AX = mybir.AxisListType
F32 = mybir.dt.float32
F32R = mybir.dt.float32r
BF16 = mybir.dt.bfloat16
I32 = mybir.dt.int32
U32 = mybir.dt.uint32


class V:
    __slots__ = ("ap", "keys")

    def __init__(self, ap, keys):
        self.ap = ap
        self.keys = keys


class T:
    def __init__(self, name, t, nsub=0):
        self.name = name
        self.t = t
        self.nsub = nsub

    def _allkeys(self):
        if self.nsub:
            return [(self.name, i) for i in range(self.nsub)]
        return [(self.name,)]

    def __getitem__(self, idx):
        return V(self.t[idx], self._allkeys())

    def k(self, i):
        tt = self

        class _S:
            def __getitem__(s, idx):
                if isinstance(i, (list, tuple, range)):
                    return V(tt.t[idx], [(tt.name, j) for j in i])
                return V(tt.t[idx], [(tt.name, i)])
        return _S()

    def v(self, ap, subs=None):
        if subs is None:
            return V(ap, self._allkeys())
        return V(ap, [(self.name, j) for j in subs])


class P:
    ENG = ("pe", "act", "dve", "pool", "sp")

    def __init__(self, ndma_sems=24):
        nc = bass.Bass("TRN2", target_bir_lowering=False)
        self.nc = nc
        self.e = {"pe": nc.tensor, "act": nc.scalar, "dve": nc.vector,
                  "pool": nc.gpsimd, "sp": nc.sync}
        self.sem = {k: nc.alloc_semaphore("s_" + k) for k in self.ENG}
        self.cnt = {k: 0 for k in self.ENG}
        self.epoch = {k: 0 for k in self.ENG}
        self.LIMIT = 20000
        self.seen = {k: {} for k in self.ENG}
        self.lastw = {}
        self.reads = {}
        self.dsem = [nc.alloc_semaphore("d%d" % i) for i in range(ndma_sems)]
        self.dval = [0] * ndma_sems
        self.dnext = 0
        self.semobj = {}
        for k in self.ENG:
            self.semobj[("e", k, 0)] = self.sem[k]
        for i, s in enumerate(self.dsem):
            self.semobj[("d", i)] = s
        self.n_inst = 0
        self.out_tokens = []
        self._stack = []
        self.last_tok = {}

    def sb(self, name, shape, dt=F32, nsub=0):
        if self._stack:
            self._uid = getattr(self, "_uid", 0) + 1
            name = "%s_u%d" % (name, self._uid)
            t = self._stack[-1].enter_context(self.nc.sbuf_tensor(name, list(shape), dt))
            return T(name, t, nsub)
        return T(name, self.nc.alloc_sbuf_tensor(name, list(shape), dt), nsub)

    def scope(self):
        import contextlib
        pp = self

        class _Sc:
            def __enter__(s):
                pp._stack.append(contextlib.ExitStack())
                return s

            def __exit__(s, *a):
                pp.barrier()
                pp._stack.pop().close()
                return False
        return _Sc()

    def barrier(self):
        toks = list(self.last_tok.values())
        for i in range(len(self.dsem)):
            if self.dval[i] > 0:
                toks.append((("d", i), self.dval[i]))
        for eng in self.ENG:
            e = self.e[eng]
            seen = self.seen[eng]
            for sid, val in toks:
                if sid[0] == "e" and sid[1] == eng:
                    continue
                if seen.get(sid, 0) < val:
                    e.wait_ge(self.semobj[sid], val)
                    seen[sid] = val

    def ps(self, name, shape, dt=F32, nsub=0):
        return T(name, self.nc.alloc_psum_tensor(name, list(shape), dt), nsub)

    def dram(self, name, shape, dt=F32, kind="Internal", nsub=0):
        return T(name, self.nc.dram_tensor(name, list(shape), dt, kind=kind), nsub)

    def _need(self, eng, reads, writes, self_raw=True):
        toks = []
        for v in reads:
            for k in v.keys:
                w = self.lastw.get(k)
                if w is not None:
                    toks.append(w)
        for v in writes:
            for k in v.keys:
                w = self.lastw.get(k)
                if w is not None:
                    toks.append(w)
                toks.extend(self.reads.get(k, ()))
        need = {}
        for (sid, val, kind) in toks:
            if sid[0] == "e" and sid[1] == eng:
                if eng == "pe" or not self_raw or kind == "r":
                    continue
            if need.get(sid, 0) < val:
                need[sid] = val
        seen = self.seen[eng]
        out = []
        for sid, val in need.items():
            if seen.get(sid, 0) < val:
                out.append((sid, val))
                seen[sid] = val
        return out

    def _record(self, tok_w, tok_r, reads, writes):
        for v in writes:
            for k in v.keys:
                self.lastw[k] = tok_w
                self.reads[k] = []
        for v in reads:
            for k in v.keys:
                self.reads.setdefault(k, []).append(tok_r)

    def op(self, eng, fn, writes, reads, self_raw=True):
        e = self.e[eng]
        for sid, val in self._need(eng, reads, writes, self_raw):
            e.wait_ge(self.semobj[sid], val)
        ins = fn(e)
        if self.cnt[eng] >= self.LIMIT:
            self.epoch[eng] += 1
            self.cnt[eng] = 0
            self.sem[eng] = self.nc.alloc_semaphore("s_%s_%d" % (eng, self.epoch[eng]))
            self.semobj[("e", eng, self.epoch[eng])] = self.sem[eng]
        self.cnt[eng] += 1
        c = self.cnt[eng]
        ins.then_inc(self.sem[eng], 1)
        sid = ("e", eng, self.epoch[eng])
        self.last_tok[eng] = (sid, c)
        self._record((sid, c, "w"), (sid, c, "r"), reads, writes)
        self.n_inst += 1
        return ins

    def dma(self, q, out, in_, is_output=False, **kw):
        e = self.e[q]
        i = self.dnext
        self.dnext = (self.dnext + 1) % len(self.dsem)
        sid = ("d", i)
        need = self._need(q, [in_], [out])
        if self.dval[i] > 0 and self.seen[q].get(sid, 0) < self.dval[i]:
            need.append((sid, self.dval[i]))
            self.seen[q][sid] = self.dval[i]
        for s, val in need:
            e.wait_ge(self.semobj[s], val)
        self.dval[i] += 16
        ins = e.dma_start(out=out.ap, in_=in_.ap, **kw)
        ins.then_inc(self.dsem[i], 16)
        tok = (sid, self.dval[i], "w")
        self._record(tok, (sid, self.dval[i], "r"), [in_], [out])
        if is_output:
            self.out_tokens.append(tok)
        self.n_inst += 1
        return ins

    def finish(self):
        e = self.e["sp"]
        for i, s in enumerate(self.dsem):
            if self.dval[i] > 0:
                e.wait_ge(s, self.dval[i])
        for k in self.ENG:
            if k != "sp" and self.cnt[k] > 0:
                e.wait_ge(self.sem[k], self.cnt[k])
        return self.nc

    def mm(self, out, lhsT, rhs, start=True, stop=True, **kw):
        return self.op("pe", lambda e: e.matmul(out.ap, lhsT.ap, rhs.ap, start=start, stop=stop, **kw),
                       [out], [lhsT, rhs])

    def tr(self, out, in_, ident):
        return self.op("pe", lambda e: e.transpose(out.ap, in_.ap, ident.ap), [out], [in_, ident])

    def act(self, out, in_, func, bias=None, scale=None, accum_out=None, eng="act"):
        kw = {}
        rd = [in_]
        wr = [out]
        if bias is not None:
            if isinstance(bias, V):
                kw["bias"] = bias.ap
                rd.append(bias)
            else:
                kw["bias"] = bias
        if scale is not None:
            if isinstance(scale, V):
                kw["scale"] = scale.ap
                rd.append(scale)
            else:
                kw["scale"] = scale
        if accum_out is not None:
            kw["accum_out"] = accum_out.ap
            wr.append(accum_out)
        return self.op(eng, lambda e: e.activation(out.ap, in_.ap, func, **kw), wr, rd)

    def tt(self, out, a, b, op, eng="dve"):
        return self.op(eng, lambda e: e.tensor_tensor(out.ap, a.ap, b.ap, op), [out], [a, b])

    def ts(self, out, a, s1, op0, s2=None, op1=None, eng="dve", accum_out=None):
        rd = [a]
        wr = [out]
        s1a = s1.ap if isinstance(s1, V) else s1
        s2a = s2.ap if isinstance(s2, V) else s2
        if isinstance(s1, V):
            rd.append(s1)
        if isinstance(s2, V):
            rd.append(s2)
        kw = {}
        if op1 is not None:
            kw["op1"] = op1
        if accum_out is not None:
            kw["accum_out"] = accum_out.ap
            wr.append(accum_out)
        return self.op(eng, lambda e: e.tensor_scalar(out.ap, a.ap, s1a, s2a, op0, **kw), wr, rd)

    def stt(self, out, a, s, b, op0, op1, eng="dve"):
        rd = [a, b]
        sa = s.ap if isinstance(s, V) else s
        if isinstance(s, V):
            rd.append(s)
        return self.op(eng, lambda e: e.scalar_tensor_tensor(out.ap, a.ap, sa, b.ap, op0, op1), [out], rd)

    def copy(self, out, in_, eng="dve"):
        if eng == "act":
            return self.op("act", lambda e: e.copy(out.ap, in_.ap), [out], [in_])
        return self.op(eng, lambda e: e.tensor_copy(out.ap, in_.ap), [out], [in_])

    def memset(self, out, val, eng="dve"):
        return self.op(eng, lambda e: e.memset(out.ap, val), [out], [])

    def reduce(self, out, in_, op, axis=AX.X, eng="dve"):
        return self.op(eng, lambda e: e.tensor_reduce(out.ap, in_.ap, axis, op), [out], [in_])

    def recip(self, out, in_):
        return self.op("dve", lambda e: e.reciprocal(out.ap, in_.ap), [out], [in_])


D = 2048
KC = 16
NTOK = 2048
TG = 512
NTG = NTOK // TG
MEMT = 256
NEXP = 64
FF = 512
EPS = 1e-6


class Banks:
    def __init__(self, p, n=8):
        self.b = [p.ps("bank%d" % i, [128, 512], F32) for i in range(n)]
        self.i = 0

    def get(self):
        b = self.b[self.i]
        self.i = (self.i + 1) % len(self.b)
        return b


class Ring:
    def __init__(self, p, name, shape, dt, n):
        self.b = [p.sb("%s%d" % (name, i), shape, dt) for i in range(n)]
        self.i = 0

    def get(self):
        b = self.b[self.i]
        self.i = (self.i + 1) % len(self.b)
        return b


def dv(t, ap):
    return V(ap, t._allkeys())


def rmsnorm_F(p, banks, xT, ntok, g_sb, gl, out_bf, ones_f32, sqring, eps_t, rstd_out):
    ps = banks.get()
    for k in range(KC):
        sq = sqring.get()
        p.act(sq[:, 0:ntok], xT[:, k, 0:ntok], AF.Square)
        p.mm(ps[:, 0:ntok], ones_f32[:, :], sq[:, 0:ntok], start=(k == 0), stop=(k == KC - 1))
    p.act(rstd_out[:, 0:ntok], ps[:, 0:ntok], AF.Sqrt, bias=eps_t[:, 0:1], scale=1.0 / D)
    p.recip(rstd_out[:, 0:ntok], rstd_out[:, 0:ntok])
    if out_bf is not None:
        for k in range(KC):
            p.stt(out_bf[:, k, 0:ntok], xT[:, k, 0:ntok], g_sb[:, gl, k:k + 1], rstd_out[:, 0:ntok],
                  ALU.mult, ALU.mult)


def linear_F(p, banks, wring, W, wl, inT, ntok, consume, ccs=range(KC), kcn=KC):
    for cc in ccs:
        wt = wring.get()
        src = W.t[wl].rearrange("(k p) c -> p k c", p=128)[:, :, cc * 128:(cc + 1) * 128]
        p.dma("pool", wt[:, 0:kcn, :], dv(W, src))
        ps = banks.get()
        for k in range(kcn):
            p.mm(ps[:, 0:ntok], wt[:, k, :], inT[:, k, 0:ntok], start=(k == 0), stop=(k == kcn - 1))
        consume(cc, ps)


def build_B(final_norm, has_mix=True, ntg=NTG, nexp=NEXP, do_cross=True, do_moe=True):
    p = P()
    nc = p.nc
    xT_d = p.dram("xT", [D, NTOK], F32, kind="ExternalInput")
    ycT_d = p.dram("ycT", [D, NTOK], F32, kind="ExternalInput")
    memT_d = p.dram("memT", [D, MEMT], F32, kind="ExternalInput")
    w_out = p.dram("w_out", [1, D, D], F32, kind="ExternalInput")
    ca_wq = p.dram("ca_wq", [1, D, D], F32, kind="ExternalInput")
    ca_wk = p.dram("ca_wk", [1, D, D], F32, kind="ExternalInput")
    ca_wv = p.dram("ca_wv", [1, D, D], F32, kind="ExternalInput")
    ca_wo = p.dram("ca_wo", [1, D, D], F32, kind="ExternalInput")
    gains = p.dram("gains", [4, D], F32, kind="ExternalInput")
    wr_d = p.dram("wr", [D, 72], F32, kind="ExternalInput")
    br_d = p.dram("br", [72], F32, kind="ExternalInput")
    if do_moe:
        wgate = p.dram("wgate", [nexp, D, FF], F32, kind="ExternalInput")
        wup = p.dram("wup", [nexp, D, FF], F32, kind="ExternalInput")
        wdown = p.dram("wdown", [nexp, FF, D], F32, kind="ExternalInput")
    ident_d = p.dram("ident_in", [128, 128], F32, kind="ExternalInput")
    outT_d = p.dram("outT", [D, NTOK], F32, kind="ExternalOutput")

    banks = Banks(p)
    ident = p.sb("ident", [128, 128], F32)
    p.dma("sp", ident[:], ident_d[:])
    ones_f = p.sb("ones_f", [128, 128], F32)
    p.memset(ones_f[:], 1.0)
    ones_b = p.sb("ones_b", [128, 128], BF16)
    p.memset(ones_b[:], 1.0)
    eps_t = p.sb("eps_t", [128, 1], F32)
    p.memset(eps_t[:], EPS)
    g_sb = p.sb("g_sb", [128, 4, KC], F32)
    for l in range(4):
        p.dma("sp", g_sb[:, l, :], dv(gains, gains.t[l].rearrange("(k p) -> p k", p=128)),
              allow_slow_non_contiguous=True)
    wr_sb = p.sb("wr_sb", [128, KC, 72], F32)
    p.dma("sp", wr_sb[:], dv(wr_d, wr_d.t[:].rearrange("(k p) c -> p k c", p=128)))
    for k in range(KC):
        p.ts(wr_sb[:, k, :], wr_sb[:, k, :], g_sb[:, 2, k:k + 1], ALU.mult)
    br_bc = p.sb("br_bc", [128, 72], F32)
    p.dma("sp", br_bc[:], dv(br_d, br_d.t[:].partition_broadcast(128)))

    xT = p.sb("xTs", [128, KC, TG], F32, nsub=KC)
    xn = p.sb("xn", [128, KC, TG], BF16)
    act2 = p.sb("act2", [128, KC, TG], BF16)
    rstd = p.sb("rstd", [128, TG], F32)
    sqring = Ring(p, "sq", [128, TG], F32, 2)
    wring = Ring(p, "wr_", [128, KC, 128], BF16, 6)
    memT = xT
    mn = xn
    kT = p.sb("kT", [128, KC, MEMT], BF16)
    vT = act2
    vtm = p.sb("vtm", [128, 2, D], BF16)
    ebuf = Ring(p, "eb", [128, 2, TG], BF16, 2)
    rden = p.sb("rden", [128, TG], F32)

    p.dma("sp", memT[:, :, 0:MEMT], dv(memT_d, memT_d.t[:].rearrange("(k p) m -> p k m", p=128)))
    rmsnorm_F(p, banks, memT, MEMT, g_sb, 1, mn, ones_f, sqring, eps_t, rstd)

    def cons_k(cc, ps):
        p.copy(kT[:, cc, :], ps[:, 0:MEMT], eng="act")
    linear_F(p, banks, wring, ca_wk, 0, mn, MEMT, cons_k)

    def cons_v(cc, ps):
        p.copy(vT[:, cc, 0:MEMT], ps[:, 0:MEMT], eng="act")
    linear_F(p, banks, wring, ca_wv, 0, mn, MEMT, cons_v)
    ident_b = p.sb("ident_b", [128, 128], BF16)
    p.copy(ident_b[:], ident[:])
    for cc in range(KC):
        for mh in range(2):
            ps = banks.get()
            psb = V(ps.t[:].bitcast(BF16)[:, 0:128], ps[:].keys)
            p.tr(psb, vT[:, cc, mh * 128:(mh + 1) * 128], ident_b[:])
            p.copy(vtm[:, mh, cc * 128:(cc + 1) * 128], psb)

    wd_ring = Ring(p, "wd", [128, 4, 512], BF16, 3)
    hid = p.sb("hid", [128, 4, TG], BF16, nsub=4)
    htmp = Ring(p, "ht", [128, TG], F32, 2)
    cwT = p.sb("cwT", [64, TG], F32)
    cwb_ring = Ring(p, "cwb", [128, TG], F32, 2)
    rs_t = p.sb("rs_t", [128, 1], F32)
    sm = Ring(p, "sm", [128, 160], F32, 2)
    lgs = Ring(p, "lgs", [128, 72], F32, 2)

    for tg in range(ntg):
        t0 = tg * TG
        xsrc = xT_d.t[:].rearrange("(k p) t -> p k t", p=128)[:, :, t0:t0 + TG]
        for k in range(KC):
            p.dma("sp", xT.k(k)[:, k, :], dv(xT_d, xsrc[:, k, :]))
        if has_mix:
            ysrc = ycT_d.t[:].rearrange("(k p) t -> p k t", p=128)[:, :, t0:t0 + TG]
            p.dma("pool", act2[:], dv(ycT_d, ysrc))

            def cons_o(cc, ps):
                p.tt(xT.k(cc)[:, cc, :], xT.k(cc)[:, cc, :], ps[:, :], ALU.add)
            linear_F(p, banks, wring, w_out, 0, act2, TG, cons_o)
        if do_cross:
            rmsnorm_F(p, banks, xT, TG, g_sb, 0, xn, ones_f, sqring, eps_t, rstd)

            def cons_q(cc, ps):
                p.act(act2[:, cc, :], ps[:, :], AF.Copy, scale=float(512 ** -0.5))
            linear_F(p, banks, wring, ca_wq, 0, xn, TG, cons_q)
            for h in range(4):
                eb = ebuf.get()
                for mh in range(2):
                    ps = banks.get()
                    for j in range(4):
                        dc = 4 * h + j
                        p.mm(ps[:, :], kT[:, dc, mh * 128:(mh + 1) * 128], act2[:, dc, :], start=(j == 0), stop=(j == 3))
                    p.act(eb[:, mh, :], ps[:, :], AF.Exp)
                psd = banks.get()
                for mh in range(2):
                    p.mm(psd[:, :], ones_b[:, :], eb[:, mh, :], start=(mh == 0), stop=(mh == 1))
                p.recip(rden[:, :], psd[:, :])
                for j in range(4):
                    dc = 4 * h + j
                    ps = banks.get()
                    for mh in range(2):
                        p.mm(ps[:, :], vtm[:, mh, dc * 128:(dc + 1) * 128], eb[:, mh, :], start=(mh == 0), stop=(mh == 1))
                    p.tt(xn[:, dc, :], ps[:, :], rden[:, :], ALU.mult)

            def cons_wo(cc, ps):
                p.tt(xT.k(cc)[:, cc, :], xT.k(cc)[:, cc, :], ps[:, :], ALU.add)
            linear_F(p, banks, wring, ca_wo, 0, xn, TG, cons_wo)
        if do_moe:
            rmsnorm_F(p, banks, xT, TG, g_sb, 2, xn, ones_f, sqring, eps_t, rstd)
            for tt in range(TG // 128):
                ts_ = slice(tt * 128, (tt + 1) * 128)
                ps = banks.get()
                for k in range(KC):
                    p.mm(ps[:, 0:72], xT.k(k)[:, k, ts_], wr_sb[:, k, :], start=(k == 0), stop=(k == KC - 1))
                pst = banks.get()
                p.tr(pst[:, 0:128], rstd[:, ts_], ident[:])
                p.copy(rs_t[:, :], pst[:, 0:1])
                lg = lgs.get()
                p.stt(lg[:, :], ps[:, 0:72], rs_t[:, 0:1], br_bc[:, :], ALU.mult, ALU.add)
                s = sm.get()
                p.reduce(s[:, 0:1], lg[:, 0:8], ALU.max)
                p.ts(s[:, 16:24], lg[:, 0:8], s[:, 0:1], ALU.is_equal)
                p.ts(s[:, 1:2], s[:, 0:1], -1.0, ALU.mult)
                p.act(s[:, 24:32], lg[:, 0:8], AF.Exp, bias=s[:, 1:2], accum_out=s[:, 2:3])
                p.recip(s[:, 3:4], s[:, 2:3])
                ohb = V(s.t[:, 16:24].unsqueeze(2).broadcast_to([128, 8, 8]), s[:].keys)
                tmp3 = V(s.t[:, 32:96].rearrange("p (g e) -> p g e", g=8), s[:].keys)
                le3 = V(lg.t[:, 8:72].rearrange("p (g e) -> p g e", g=8), lg[:].keys)
                p.tt(tmp3, le3, ohb, ALU.mult)
                tmpT = V(s.t[:, 32:96].rearrange("p (g e) -> p e g", g=8), s[:].keys)
                p.reduce(s[:, 96:104], tmpT, ALU.add)
                p.reduce(s[:, 4:5], s[:, 96:104], ALU.max)
                p.ts(s[:, 104:112], s[:, 96:104], s[:, 4:5], ALU.is_equal)
                p.stt(s[:, 112:120], s[:, 104:112], -1e30, s[:, 96:104], ALU.mult, ALU.add)
                p.reduce(s[:, 5:6], s[:, 112:120], ALU.max)
                p.ts(s[:, 120:128], s[:, 112:120], s[:, 5:6], ALU.is_equal)
                p.tt(s[:, 6:7], s[:, 5:6], s[:, 4:5], ALU.subtract)
                p.act(s[:, 7:8], s[:, 6:7], AF.Exp)
                p.ts(s[:, 8:9], s[:, 7:8], 1.0, ALU.add)
                p.recip(s[:, 8:9], s[:, 8:9])
                p.tt(s[:, 9:10], s[:, 7:8], s[:, 8:9], ALU.mult)
                p.tt(s[:, 8:9], s[:, 8:9], s[:, 3:4], ALU.mult)
                p.tt(s[:, 9:10], s[:, 9:10], s[:, 3:4], ALU.mult)
                p.ts(s[:, 128:136], s[:, 104:112], s[:, 8:9], ALU.mult)
                p.stt(s[:, 136:144], s[:, 120:128], s[:, 9:10], s[:, 128:136], ALU.mult, ALU.add)
                cwgb = V(s.t[:, 136:144].unsqueeze(1).broadcast_to([128, 8, 8]), s[:].keys)
                p.tt(tmp3, ohb, cwgb, ALU.mult)
                pc = banks.get()
                p.tr(pc[0:64, 0:128], s[:, 32:96], ident[:])
                p.copy(cwT[:, ts_], pc[0:64, 0:128])
            for e in range(nexp):
                pb = banks.get()
                sel = V(ident.t[0:64, e:e + 1].broadcast_to([64, 128]), ident[:].keys)
                p.mm(pb[:, :], sel, cwT[:, :])
                cwb = cwb_ring.get()
                p.copy(cwb[:, :], pb[:, :], eng="act")
                for f in range(4):
                    wg = wring.get()
                    p.dma("pool", wg[:], dv(wgate, wgate.t[e].rearrange("(k p) f -> p k f", p=128)[:, :, f * 128:(f + 1) * 128]))
                    wu = wring.get()
                    p.dma("pool", wu[:], dv(wup, wup.t[e].rearrange("(k p) f -> p k f", p=128)[:, :, f * 128:(f + 1) * 128]))
                    pg = banks.get()
                    for k in range(KC):
                        p.mm(pg[:, :], wg[:, k, :], xn[:, k, :], start=(k == 0), stop=(k == KC - 1))
                    pu = banks.get()
                    for k in range(KC):
                        p.mm(pu[:, :], wu[:, k, :], xn[:, k, :], start=(k == 0), stop=(k == KC - 1))
                    ht = htmp.get()
                    p.act(ht[:, :], pg[:, :], AF.Silu)
                    p.tt(ht[:, :], ht[:, :], pu[:, :], ALU.mult)
                    p.tt(hid.k(f)[:, f, :], ht[:, :], cwb[:, :], ALU.mult)
                for cc in range(KC):
                    if cc % 4 == 0:
                        wd = wd_ring.get()
                        p.dma("pool", wd[:], dv(wdown, wdown.t[e].rearrange("(f p) c -> p f c", p=128)[:, :, cc * 128:cc * 128 + 512]))
                    pd = banks.get()
                    for f in range(4):
                        p.mm(pd[:, :], wd[:, f, (cc % 4) * 128:(cc % 4 + 1) * 128], hid.k(f)[:, f, :], start=(f == 0), stop=(f == 3))
                    p.tt(xT.k(cc)[:, cc, :], xT.k(cc)[:, cc, :], pd[:, :], ALU.add)
        odst = outT_d.t[:].rearrange("(k p) t -> p k t", p=128)[:, :, t0:t0 + TG]
        if final_norm:
            rmsnorm_F(p, banks, xT, TG, g_sb, 3, None, ones_f, sqring, eps_t, rstd)
            for k in range(KC):
                p.stt(xT.k(k)[:, k, :], xT.k(k)[:, k, :], g_sb[:, 3, k:k + 1], rstd[:, :], ALU.mult, ALU.mult)
        for k in range(KC):
            p.dma("sp", dv(outT_d, odst[:, k, :]), xT.k(k)[:, k, :], is_output=True)
    p.finish()
    return p


D = 2048
KC = 16
S = 4096
NFC = 20
TB = 512
EPS = 1e-6
ML_L = 128
RW_L = 64
RW_SB = 128
NEG = -1e30


def build_A(do_ml=True, do_rw=True, s_len=S):
    p = P()
    SL = s_len
    xT_d = p.dram("xT", [D, SL], F32, kind="ExternalInput")
    WF = p.dram("WF", [1, D, NFC * 128], F32, kind="ExternalInput")
    WT = p.dram("WT", [D, 1024], F32, kind="ExternalInput")
    gmix = p.dram("gmix", [1, D], F32, kind="ExternalInput")
    convw = p.dram("convw", [128, 4, 4], F32, kind="ExternalInput")
    convb = p.dram("convb", [128, 4], F32, kind="ExternalInput")
    gateb = p.dram("gateb", [1, 4], F32, kind="ExternalInput")
    rwvec = p.dram("rwvec", [64, 10, 8], F32, kind="ExternalInput")
    mulora = p.dram("mulora", [128, 3], F32, kind="ExternalInput")
    wup = p.dram("wup", [64, 512], F32, kind="ExternalInput")
    aup = p.dram("aup", [64, 512], F32, kind="ExternalInput")
    gup = p.dram("gup", [128, 512], F32, kind="ExternalInput")
    ident_d = p.dram("ident_in", [128, 128], F32, kind="ExternalInput")
    mlmask_d = p.dram("mlmask", [128, 128], F32, kind="ExternalInput")
    rwmask_d = p.dram("rwmask", [64, 5, 64], F32, kind="ExternalInput")
    ym_d = p.dram("ym", [SL, 512], F32, kind="ExternalOutput")
    yr_d = p.dram("yrT", [512, SL], F32, kind="ExternalOutput")
    scF = p.dram("scF", [NFC * 128, SL], F32)
    scT = p.dram("scT", [SL, 1024], F32)
    scG = p.dram("scG", [2, SL], F32)

    banks = Banks(p)
    ident = p.sb("ident", [128, 128], F32)
    p.dma("sp", ident[:], ident_d[:])
    ones_f = p.sb("ones_f", [128, 128], F32)
    p.memset(ones_f[:], 1.0)
    eps_t = p.sb("eps_t", [128, 1], F32)
    p.memset(eps_t[:], EPS)
    g_sb = p.sb("g_sb", [128, 1, KC], F32)
    p.dma("sp", g_sb[:, 0, :], dv(gmix, gmix.t[0].rearrange("(k p) -> p k", p=128)), allow_slow_non_contiguous=True)

    sc1 = p.scope()
    sc1.__enter__()
    xT = p.sb("xTs", [128, KC, TB], F32)
    xn = p.sb("xn", [128, KC, TB], BF16)
    rstd = p.sb("rstd", [128, TB], F32)
    sqring = Ring(p, "sq", [128, TB], F32, 2)
    wring = Ring(p, "wr_", [128, KC, 128], BF16, 4)
    wtring = Ring(p, "wt_", [128, KC, 512], BF16, 2)
    stage = Ring(p, "stg", [128, TB], F32, 4)
    for tb in range(SL // TB):
        t0 = tb * TB
        p.dma("sp", xT[:], dv(xT_d, xT_d.t[:].rearrange("(k p) t -> p k t", p=128)[:, :, t0:t0 + TB]))
        rmsnorm_F(p, banks, xT, TB, g_sb, 0, xn, ones_f, sqring, eps_t, rstd)

        def cons_f(cc, ps, t0=t0):
            st = stage.get()
            p.copy(st[:, :], ps[:, :], eng="act")
            p.dma("sp", dv(scF, scF.t[cc * 128:(cc + 1) * 128, t0:t0 + TB]), st[:, :])
        linear_F(p, banks, wring, WF, 0, xn, TB, cons_f, ccs=range(NFC))
        for cg in range(2):
            wt = wtring.get()
            p.dma("pool", wt[:], dv(WT, WT.t[:].rearrange("(k p) c -> p k c", p=128)[:, :, cg * 512:(cg + 1) * 512]))
            for tt in range(TB // 128):
                ps = banks.get()
                for k in range(KC):
                    p.mm(ps[:, :], xn[:, k, tt * 128:(tt + 1) * 128], wt[:, k, :], start=(k == 0), stop=(k == KC - 1))
                st = stage.get()
                p.copy(st[:, :], ps[:, :], eng="act")
                p.dma("sp", dv(scT, scT.t[t0 + tt * 128:t0 + (tt + 1) * 128, cg * 512:(cg + 1) * 512]), st[:, :])
    sc1.__exit__(None, None, None)

    if do_ml:
      with p.scope():
        NCH = SL // ML_L
        cw_sb = p.sb("cw_sb", [128, 4, 4], F32)
        p.dma("sp", cw_sb[:], convw[:])
        cb_sb = p.sb("cb_sb", [128, 4], F32)
        p.dma("sp", cb_sb[:], convb[:])
        mlmask = p.sb("mlmask_s", [128, 128], F32)
        p.dma("sp", mlmask[:], mlmask_d[:])
        gb = p.sb("gb", [NCH, 4], F32)
        p.dma("sp", gb[:], dv(gateb, gateb.t[0].partition_broadcast(NCH)))
        ngb = p.sb("ngb", [NCH, 4], F32)
        p.ts(ngb[:], gb[:], -1.0, ALU.mult)
        raw = p.sb("ml_raw", [128, SL + 3], F32)
        acc = p.sb("ml_acc", [128, SL], F32)
        qk = [p.sb("ml_qk%d" % i, [128, SL], F32) for i in range(4)]
        for c in range(4):
            p.memset(raw[:, 0:3], 0.0)
            p.dma("sp", raw[:, 3:SL + 3], dv(scF, scF.t[c * 128:(c + 1) * 128, :]))
            p.ts(acc[:, :], raw[:, 0:SL], cw_sb[:, c, 0:1], ALU.mult, cb_sb[:, c:c + 1], ALU.add)
            for j in range(1, 4):
                p.stt(acc[:, :], raw[:, j:j + SL], cw_sb[:, c, j:j + 1], acc[:, :], ALU.mult, ALU.add)
            p.act(qk[c][:, :], acc[:, :], AF.Silu)
        G = {n: p.sb("g_" + n, [NCH, ML_L], F32) for n in ["i", "f", "b", "a", "pm", "mx", "t1", "wst", "einv", "wk", "one", "zero", "nmx", "decf"]}
        cl = p.sb("g_cols", [NCH, 8], F32)
        rw_ = p.sb("g_rows", [1, 4, NCH], F32)
        p.memset(G["one"][:], 1.0)
        p.memset(G["zero"][:], 0.0)
        A2 = p.sb("A2", [33, SL], F32)
        U2 = p.sb("U2", [33, SL], F32)
        tokc = p.sb("tokc", [128, 3, NCH], F32)
        decbc = p.sb("decbc", [128, NCH], F32)
        Cst = p.sb("Cst", [128, 257], F32)
        vring = Ring(p, "mlv", [128, 257], F32, 3)
        oring = Ring(p, "mlo", [128, 256], F32, 3)
        for vb in vring.b:
            p.memset(vb[:, 256:257], 1.0)
        wk_s = Ring(p, "ml_t", [128, 128], F32, 2)
        ex_s = Ring(p, "ml_e", [128, 128], F32, 2)
        sw_s = Ring(p, "ml_sw", [128, 128], F32, 2)
        kw_s = Ring(p, "ml_kw", [128, 128], F32, 2)
        ni_s = Ring(p, "ml_ni", [128, 257], F32, 2)
        num_s = Ring(p, "ml_num", [128, 257], F32, 2)
        den_s = Ring(p, "ml_den", [128, 2], F32, 2)
        y_s = Ring(p, "ml_y", [128, 256], F32, 2)
        p.memset(A2[:, :], 0.0)
        p.memset(U2[:, :], 0.0)
        p.memset(A2[32:33, :], 1.0)
        p.memset(U2[0:1, :], 1.0)

        def scan(out, d0, d1, init, op0, op1):
            p.op("dve", lambda e: e.tensor_tensor_scan(out.ap, d0.ap, d1.ap, init, op0, op1), [out], [d0, d1])

        for h in range(2):
            p.dma("sp", G["i"][:, :], dv(scF, scF.t[4 * 128 + h, :].rearrange("(c l) -> c l", l=ML_L)))
            p.dma("sp", G["f"][:, :], dv(scF, scF.t[4 * 128 + 2 + h, :].rearrange("(c l) -> c l", l=ML_L)))
            p.ts(G["i"][:, :], G["i"][:, :], gb[:, h:h + 1], ALU.add)
            p.act(G["t1"][:, :], G["f"][:, :], AF.Exp, bias=ngb[:, 2 + h:3 + h], scale=-1.0)
            p.ts(G["t1"][:, :], G["t1"][:, :], 1.0, ALU.add)
            p.act(G["t1"][:, :], G["t1"][:, :], AF.Ln)
            p.ts(G["f"][:, :], G["t1"][:, :], -1.0, ALU.mult)
            scan(G["b"][:, :], G["one"][:, :], G["f"][:, :], 0.0, ALU.mult, ALU.add)
            p.tt(G["a"][:, :], G["i"][:, :], G["b"][:, :], ALU.subtract)
            scan(G["pm"][:, :], G["zero"][:, :], G["a"][:, :], NEG, ALU.add, ALU.max)
            p.copy(cl[:, 0:1], G["b"][:, ML_L - 1:ML_L])
            p.copy(cl[:, 1:2], G["pm"][:, ML_L - 1:ML_L])
            for j, col in enumerate((1, 0)):
                pt = banks.get()
                p.tr(pt[0:1, 0:NCH], cl[:, col:col + 1], ident[0:NCH, 0:NCH])
                p.copy(rw_[:, j, :], pt[0:1, 0:NCH])
            scan(rw_[:, 2, :], rw_[:, 0, :], rw_[:, 1, :], 0.0, ALU.max, ALU.add)
            p.memset(rw_[:, 3, 0:1], 0.0)
            p.copy(rw_[:, 3, 1:NCH], rw_[:, 2, 0:NCH - 1])
            for j, col in ((2, 2), (3, 3)):
                pt = banks.get()
                p.tr(pt[0:NCH, 0:1], rw_[:, j, :], ident[0:1, 0:1])
                p.copy(cl[:, col:col + 1], pt[0:NCH, 0:1])
            p.ts(cl[:, 6:7], cl[:, 3:4], -1.0, ALU.mult)
            p.ts(G["mx"][:, :], G["pm"][:, :], cl[:, 3:4], ALU.max)
            p.act(G["wst"][:, :], G["mx"][:, :], AF.Exp, bias=cl[:, 3:4], scale=-1.0)
            p.ts(G["wst"][:, :], G["wst"][:, :], float(128 ** -0.5), ALU.mult)
            p.tt(G["t1"][:, :], G["b"][:, :], G["mx"][:, :], ALU.add)
            p.act(G["einv"][:, :], G["t1"][:, :], AF.Exp, scale=-1.0)
            p.tt(cl[:, 4:5], cl[:, 0:1], cl[:, 2:3], ALU.subtract)
            p.act(G["wk"][:, :], G["a"][:, :], AF.Exp, bias=cl[:, 4:5])
            p.tt(cl[:, 5:6], cl[:, 4:5], cl[:, 3:4], ALU.add)
            p.act(cl[:, 5:6], cl[:, 5:6], AF.Exp)
            p.ts(G["nmx"][:, :], G["mx"][:, :], -1.0, ALU.mult)
            p.ts(G["decf"][:, :], G["one"][:, :], cl[:, 5:6], ALU.mult)
            p.dma("sp", dv(scG, scG.t[0, :].rearrange("(c l) -> c l", l=ML_L)), G["a"][:, :])
            p.dma("sp", dv(scG, scG.t[1, :].rearrange("(c l) -> c l", l=ML_L)), G["nmx"][:, :])
            p.dma("sp", A2[0:1, :], dv(scG, scG.t[0:1, :]))
            p.dma("sp", U2[32:33, :], dv(scG, scG.t[1:2, :]))
            for j, nm in enumerate(("wst", "einv", "wk")):
                pt = banks.get()
                p.tr(pt[:, 0:NCH], G[nm][:, :], ident[0:NCH, 0:NCH])
                p.copy(tokc[:, j, :], pt[:, 0:NCH])
            pt = banks.get()
            p.tr(pt[:, 0:NCH], G["decf"][:, :], ident[0:NCH, 0:NCH])
            p.copy(decbc[:, :], pt[:, 0:NCH])
            p.memset(Cst[:, :], 0.0)
            qT, kT = qk[h], qk[2 + h]
            for c in range(NCH):
                cs = slice(c * ML_L, (c + 1) * ML_L)
                vt = vring.get()
                p.dma("sp", vt[:, 0:256], dv(scT, scT.t[cs, h * 256:(h + 1) * 256]))
                ot = oring.get()
                p.dma("sp", ot[:, :], dv(scT, scT.t[cs, 512 + h * 256:512 + (h + 1) * 256]))
                ps_s = banks.get()
                p.mm(ps_s[:, 0:128], kT[:, cs], qT[:, cs])
                ps_d = banks.get()
                p.mm(ps_d[:, 0:128], A2[:, cs], U2[:, cs])
                t = wk_s.get()
                p.tt(t[:, :], ps_d[:, 0:128], mlmask[:, :], ALU.add)
                ex = ex_s.get()
                p.act(ex[:, :], t[:, :], AF.Exp)
                sw = sw_s.get()
                p.tt(sw[:, :], ps_s[:, 0:128], ex[:, :], ALU.mult)
                p.ts(sw[:, :], sw[:, :], float(128 ** -0.5), ALU.mult)
                ps_i = banks.get()
                p.mm(ps_i[:, 0:257], sw[:, :], vt[:, :])
                ps_c = banks.get()
                p.mm(ps_c[:, 0:257], qT[:, cs], Cst[:, :])
                ni = ni_s.get()
                p.copy(ni[:, :], ps_i[:, 0:257], eng="act")
                num = num_s.get()
                p.stt(num[:, :], ps_c[:, 0:257], tokc[:, 0, c:c + 1], ni[:, :], ALU.mult, ALU.add)
                dn = den_s.get()
                p.ts(dn[:, 0:1], num[:, 256:257], -1.0, ALU.mult)
                p.tt(dn[:, 0:1], dn[:, 0:1], num[:, 256:257], ALU.max)
                p.ts(dn[:, 0:1], dn[:, 0:1], tokc[:, 1, c:c + 1], ALU.max)
                p.recip(dn[:, 1:2], dn[:, 0:1])
                p.act(ot[:, :], ot[:, :], AF.Sigmoid)
                y = y_s.get()
                p.stt(y[:, :], num[:, 0:256], dn[:, 1:2], ot[:, :], ALU.mult, ALU.mult)
                p.dma("sp", dv(ym_d, ym_d.t[cs, h * 256:(h + 1) * 256]), y[:, :], is_output=True)
                ps_k = banks.get()
                p.tr(ps_k[:, 0:128], kT[:, cs], ident[:, :])
                kw = kw_s.get()
                p.act(kw[:, :], ps_k[:, 0:128], AF.Copy, scale=tokc[:, 2, c:c + 1])
                ps_dc = banks.get()
                p.mm(ps_dc[:, 0:257], kw[:, :], vt[:, :])
                p.stt(Cst[:, :], Cst[:, :], decbc[:, c:c + 1], ps_dc[:, 0:257], ALU.mult, ALU.add)
    else:
        zt = p.sb("zt", [128, 512], F32)
        p.memset(zt[:], 0.0)
        for c in range(SL // 128):
            p.dma("sp", dv(ym_d, ym_d.t[c * 128:(c + 1) * 128, :]), zt[:, :], is_output=True)

    if do_rw:
      with p.scope():
        NH = 8
        L = RW_L
        SB = RW_SB
        NCS = SB // L
        rv = p.sb("rv", [64, 10, NH], F32)
        p.dma("sp", rv[:], rwvec[:])
        omu = p.sb("omu", [64, 3, NH], F32)
        p.ts(omu[:], rv[:, 0:3, :], -1.0, ALU.mult, 1.0, ALU.add)
        mul_ = p.sb("mul_", [128, 3], F32)
        p.dma("sp", mul_[:], mulora[:])
        omul = p.sb("omul", [128, 3], F32)
        p.ts(omul[:], mul_[:], -1.0, ALU.mult, 1.0, ALU.add)
        wup_s = p.sb("wup_s", [64, 512], F32)
        p.dma("sp", wup_s[:], wup[:])
        aup_s = p.sb("aup_s", [64, 512], F32)
        p.dma("sp", aup_s[:], aup[:])
        gup_s = p.sb("gup_s", [128, 512], F32)
        p.dma("sp", gup_s[:], gup[:])
        rwm = p.sb("rwm", [64, 5, 64], F32)
        p.dma("sp", rwm[:], rwmask_d[:])
        ones64 = p.sb("ones64", [64, 64], F32)
        p.memset(ones64[:], 1.0)
        inv64 = p.sb("inv64", [64, 64], F32)
        p.memset(inv64[:], 1.0 / 64)
        reset = p.sb("rwreset", [64, NH * SB], F32)
        p.memset(reset[:], 1.0)
        p.memset(V(reset.t[:, :].rearrange("p (x l) -> p x l", l=L)[:, :, 0:1], reset[:].keys), 0.0)
        carry = p.sb("rwcarry", [64, 3, NH], F32)
        p.memset(carry[:], 0.0)
        carl = p.sb("rwcarl", [128, 3], F32)
        p.memset(carl[:], 0.0)
        ST = p.sb("rwST", [64, NH, 64], F32)
        p.memset(ST[:], 0.0)

        def big(n, dt=F32):
            return p.sb(n, [64, NH, SB], dt)
        rawt = [p.sb("rw_raw%d" % i, [64, NH, SB + 1], F32) for i in range(3)]
        r_, k_, v_ = big("rw_r"), big("rw_k"), big("rw_v")
        lw, aa, gg, kk = big("rw_lw"), big("rw_a"), big("rw_g"), big("rw_kk")
        cc_, ee, tmp = big("rw_c"), big("rw_e"), big("rw_tmp")
        rh, kt_, bt_, kkh = big("rw_rh"), big("rw_kt"), big("rw_bt"), big("rw_kkh")
        bon = big("rw_bon")
        Y = big("rw_Y")
        lraw = p.sb("rw_lraw", [128, 3, SB + 1], F32)
        lx = p.sb("rw_lx", [128, 3, SB], F32)
        A5 = p.sb("rw_A5", [64, 5, NH * 64], F32, nsub=5)
        Nn = Ring(p, "rw_N", [64, 2, NH * 64], F32, 2)
        Pm = Ring(p, "rw_P", [64, NH * 64], F32, 2)
        TT = p.sb("rw_TT", [64, 3, NH * 64], F32, nsub=3)
        U0 = p.sb("rw_U0", [64, NH * 64], F32)
        nU = p.sb("rw_nU", [64, NH * 64], F32)

        def f2(t):
            return V(t.t[:].rearrange("p h t -> p (h t)"), t[:].keys)

        def pb(which, t=None):
            return V(rv.t[:, which, :].unsqueeze(2).broadcast_to([64, NH, SB]), rv[:].keys)

        def hv(t, h, c0, n=L):
            return V(t.t[:, h, c0:c0 + n], t[:].keys)

        def a5(i, h):
            return A5.k(i)[:, i, h * 64:(h + 1) * 64]

        for sb_ in range(SL // SB):
            t0 = sb_ * SB
            for i in range(3):
                p.copy(V(rawt[i].t[:, :, 0:1], rawt[i][:].keys), V(carry.t[:, i, :].unsqueeze(2), carry[:].keys))
                src = scF.t[(5 + 4 * i) * 128:(9 + 4 * i) * 128, t0:t0 + SB].rearrange("(h c) t -> c h t", c=64)
                p.dma("sp", V(rawt[i].t[:, :, 1:SB + 1], rawt[i][:].keys), dv(scF, src))
            for i in range(3):
                p.copy(V(carry.t[:, i, :].unsqueeze(2), carry[:].keys), V(rawt[i].t[:, :, SB:SB + 1], rawt[i][:].keys))
            p.copy(lraw[:, :, 0:1], V(carl.t[:, :].unsqueeze(2), carl[:].keys))
            for i in range(3):
                nr = 64 if i < 2 else 128
                p.dma("sp", lraw[0:nr, i, 1:SB + 1], dv(scF, scF.t[(17 + i) * 128:(17 + i) * 128 + nr, t0:t0 + SB]))
            p.copy(V(carl.t[:, :].unsqueeze(2), carl[:].keys), lraw[:, :, SB:SB + 1])
            for i, dst in enumerate((r_, k_, v_)):
                mu_b = V(rv.t[:, i, :].unsqueeze(2).broadcast_to([64, NH, SB]), rv[:].keys)
                om_b = V(omu.t[:, i, :].unsqueeze(2).broadcast_to([64, NH, SB]), omu[:].keys)
                p.tt(tmp[:], V(rawt[i].t[:, :, 0:SB], rawt[i][:].keys), mu_b, ALU.mult)
                p.tt(dst[:], V(rawt[i].t[:, :, 1:SB + 1], rawt[i][:].keys), om_b, ALU.mult)
                p.tt(dst[:], dst[:], tmp[:], ALU.add)
            for i in range(3):
                nr = 64 if i < 2 else 128
                p.ts(lx[0:nr, i, :], lraw[0:nr, i, 1:SB + 1], omul[0:nr, i:i + 1], ALU.mult)
                p.stt(lx[0:nr, i, :], lraw[0:nr, i, 0:SB], mul_[0:nr, i:i + 1], lx[0:nr, i, :], ALU.mult, ALU.add)
            p.act(lx[0:64, 0, :], lx[0:64, 0, :], AF.Tanh)
            p.act(lx[:, 2, :], lx[:, 2, :], AF.Sigmoid)
            for h in range(NH):
                ps = banks.get()
                p.mm(ps[0:64, 0:SB], wup_s[:, h * 64:(h + 1) * 64], lx[0:64, 0, :])
                p.act(lw[:, h, :], ps[0:64, 0:SB], AF.Sigmoid, bias=rv[:, 3, h:h + 1])
                ps = banks.get()
                p.mm(ps[0:64, 0:SB], aup_s[:, h * 64:(h + 1) * 64], lx[0:64, 1, :])
                p.act(aa[:, h, :], ps[0:64, 0:SB], AF.Sigmoid, bias=rv[:, 4, h:h + 1])
                ps = banks.get()
                p.mm(ps[0:64, 0:SB], gup_s[:, h * 64:(h + 1) * 64], lx[:, 2, :])
                p.copy(gg[:, h, :], ps[0:64, 0:SB], eng="act")
            p.ts(f2(lw), f2(lw), float(-math.exp(-0.5)), ALU.mult)
            p.tt(kk[:], k_[:], pb(5), ALU.mult)
            p.tt(tmp[:], kk[:], kk[:], ALU.mult)
            for q4 in range(NH * SB // 512):
                ps = banks.get()
                p.mm(ps[0:64, :], ones64[:, :], V(f2(tmp).ap[:, q4 * 512:(q4 + 1) * 512], tmp[:].keys))
                p.ts(V(f2(ee).ap[:, q4 * 512:(q4 + 1) * 512], ee[:].keys), ps[0:64, :], 1e-12, ALU.add)
            p.act(f2(ee), f2(ee), AF.Sqrt)
            p.recip(f2(ee), f2(ee))
            p.tt(kk[:], kk[:], ee[:], ALU.mult)
            p.ts(f2(tmp), f2(aa), -1.0, ALU.add)
            p.tt(tmp[:], tmp[:], pb(6), ALU.mult)
            p.ts(f2(tmp), f2(tmp), 1.0, ALU.add)
            p.tt(k_[:], k_[:], tmp[:], ALU.mult)
            p.tt(tmp[:], r_[:], k_[:], ALU.mult)
            p.tt(tmp[:], tmp[:], pb(7), ALU.mult)
            for q4 in range(NH * SB // 512):
                ps = banks.get()
                p.mm(ps[0:64, :], ones64[:, :], V(f2(tmp).ap[:, q4 * 512:(q4 + 1) * 512], tmp[:].keys))
                p.tt(V(f2(bon).ap[:, q4 * 512:(q4 + 1) * 512], bon[:].keys), ps[0:64, :],
                     V(f2(v_).ap[:, q4 * 512:(q4 + 1) * 512], v_[:].keys), ALU.mult)
            p.op("dve", lambda e: e.tensor_tensor_scan(f2(cc_).ap, reset.t[:, :], f2(lw).ap, 0.0, ALU.mult, ALU.add),
                 [cc_[:]], [reset[:], lw[:]])
            p.act(f2(ee), f2(cc_), AF.Exp)
            p.tt(rh[:], r_[:], ee[:], ALU.mult)
            gam = ee
            p.act(f2(tmp), f2(cc_), AF.Exp, scale=-1.0)
            p.tt(kt_[:], k_[:], tmp[:], ALU.mult)
            p.tt(bt_[:], kk[:], aa[:], ALU.mult)
            p.tt(bt_[:], bt_[:], tmp[:], ALU.mult)
            p.tt(tmp[:], cc_[:], lw[:], ALU.subtract)
            p.act(f2(tmp), f2(tmp), AF.Exp)
            p.tt(kkh[:], kk[:], tmp[:], ALU.mult)
            for ci in range(NCS):
                c0 = ci * L
                banksA = [banks.get() for _ in range(5)]
                for h in range(NH):
                    hs = slice(h * 64, (h + 1) * 64)
                    p.mm(banksA[0][0:64, hs], hv(kt_, h, c0), hv(kkh, h, c0))
                    p.mm(banksA[1][0:64, hs], hv(bt_, h, c0), hv(kkh, h, c0))
                    p.mm(banksA[2][0:64, hs], hv(kt_, h, c0), hv(rh, h, c0))
                    p.mm(banksA[3][0:64, hs], hv(bt_, h, c0), hv(rh, h, c0))
                    p.mm(banksA[4][0:64, hs], hv(kkh, h, c0), hv(bt_, h, c0))
                for i in range(5):
                    mk = V(rwm.t[:, i, :].unsqueeze(1).broadcast_to([64, NH, 64]), rwm[:].keys)
                    p.tt(V(A5.t[:, i, :].rearrange("p (h t) -> p h t", h=NH), [("rw_A5", i)]),
                         V(banksA[i].t[0:64, :].rearrange("p (h t) -> p h t", h=NH), banksA[i][:].keys), mk, ALU.mult,
                         eng="dve")
                for i, src in enumerate((kt_, bt_, v_)):
                    pt = banks.get()
                    for h in range(NH):
                        p.tr(pt[0:64, h * 64:(h + 1) * 64], hv(src, h, c0), ident[0:64, 0:64])
                    p.copy(TT.k(i)[:, i, :], pt[0:64, :], eng="act")
                nn = Nn.get()
                p.copy(nn[:, 0, :], A5.k(1)[:, 1, :])
                p.copy(nn[:, 1, :], A5.k(4)[:, 4, :])
                pm = Pm.get()
                identb = V(ident.t[0:64, 0:64].unsqueeze(1).broadcast_to([64, NH, 64]), ident[:].keys)
                p.tt(V(pm.t[:, :].rearrange("p (h t) -> p h t", h=NH), pm[:].keys), identb,
                     V(A5.t[:, 1, :].rearrange("p (h t) -> p h t", h=NH), [("rw_A5", 1)]), ALU.subtract)
                for lev in range(5):
                    pn = banks.get()
                    pnt = banks.get()
                    for h in range(NH):
                        hs = slice(h * 64, (h + 1) * 64)
                        p.mm(pn[0:64, hs], nn[:, 1, hs], nn[:, 0, hs])
                        p.mm(pnt[0:64, hs], nn[:, 0, hs], nn[:, 1, hs])
                    nn2 = Nn.get()
                    p.copy(nn2[:, 0, :], pn[0:64, :], eng="act")
                    p.copy(nn2[:, 1, :], pnt[0:64, :], eng="act")
                    pp = banks.get()
                    for h in range(NH):
                        hs = slice(h * 64, (h + 1) * 64)
                        p.mm(pp[0:64, hs], nn2[:, 1, hs], pm[:, hs])
                    pm2 = Pm.get()
                    p.tt(pm2[:, :], pp[0:64, :], pm[:, :], ALU.add)
                    pm = pm2
                    nn = nn2
                pu = banks.get()
                for h in range(NH):
                    hs = slice(h * 64, (h + 1) * 64)
                    p.mm(pu[0:64, hs], hv(kkh, h, c0), ST[:, h, :], start=True, stop=False)
                    p.mm(pu[0:64, hs], a5(0, h), TT.k(2)[:, 2, hs], start=False, stop=True)
                p.copy(U0[:, :], pu[0:64, :], eng="act")
                pu2 = banks.get()
                for h in range(NH):
                    hs = slice(h * 64, (h + 1) * 64)
                    p.mm(pu2[0:64, hs], pm[:, hs], U0[:, hs])
                p.ts(nU[:, :], pu2[0:64, :], -1.0, ALU.mult)
                py = banks.get()
                for h in range(NH):
                    hs = slice(h * 64, (h + 1) * 64)
                    p.mm(py[0:64, hs], ST[:, h, :], hv(rh, h, c0), start=True, stop=False)
                    p.mm(py[0:64, hs], TT.k(2)[:, 2, hs], a5(2, h), start=False, stop=False)
                    p.mm(py[0:64, hs], nU[:, hs], a5(3, h), start=False, stop=True)
                p.copy(V(Y.t[:, :, c0:c0 + L], Y[:].keys), V(py.t[0:64, :].rearrange("p (h t) -> p h t", h=NH), py[:].keys), eng="act")
                pS = banks.get()
                for h in range(NH):
                    hs = slice(h * 64, (h + 1) * 64)
                    p.mm(pS[0:64, hs], TT.k(0)[:, 0, hs], TT.k(2)[:, 2, hs], start=True, stop=False)
                    p.mm(pS[0:64, hs], TT.k(1)[:, 1, hs], nU[:, hs], start=False, stop=True)
                STf = V(ST.t[:].rearrange("p h v -> p (h v)"), ST[:].keys)
                p.tt(STf, STf, pS[0:64, :], ALU.add)
                gl = V(gam.t[:, :, c0 + L - 1:c0 + L].broadcast_to([64, NH, 64]), gam[:].keys)
                p.tt(ST[:], ST[:], gl, ALU.mult)
            for q4 in range(NH * SB // 512):
                qs = slice(q4 * 512, (q4 + 1) * 512)
                ps = banks.get()
                p.mm(ps[0:64, :], inv64[:, :], V(f2(Y).ap[:, qs], Y[:].keys))
                p.tt(V(f2(Y).ap[:, qs], Y[:].keys), V(f2(Y).ap[:, qs], Y[:].keys), ps[0:64, :], ALU.subtract)
            p.tt(tmp[:], Y[:], Y[:], ALU.mult)
            for q4 in range(NH * SB // 512):
                qs = slice(q4 * 512, (q4 + 1) * 512)
                ps = banks.get()
                p.mm(ps[0:64, :], inv64[:, :], V(f2(tmp).ap[:, qs], tmp[:].keys))
                p.ts(V(f2(cc_).ap[:, qs], cc_[:].keys), ps[0:64, :], float(64e-5), ALU.add)
            p.act(f2(cc_), f2(cc_), AF.Sqrt)
            p.recip(f2(cc_), f2(cc_))
            p.tt(Y[:], Y[:], cc_[:], ALU.mult)
            p.tt(Y[:], Y[:], pb(8), ALU.mult)
            p.tt(Y[:], Y[:], pb(9), ALU.add)
            p.tt(Y[:], Y[:], bon[:], ALU.add)
            p.tt(Y[:], Y[:], gg[:], ALU.mult)
            p.dma("sp", dv(yr_d, yr_d.t[:, t0:t0 + SB].rearrange("(h c) t -> c h t", c=64)), Y[:], is_output=True)
    else:
        zt2 = p.sb("zt2", [128, 512], F32)
        p.memset(zt2[:], 0.0)
        for c in range(4):
            for t in range(SL // 512):
                p.dma("sp", dv(yr_d, yr_d.t[c * 128:(c + 1) * 128, t * 512:(t + 1) * 512]), zt2[:, :], is_output=True)
    p.finish()
    return p


D = 2048
KC = 16
S = 4096
NSLOT = 28
TB = 512
EPS = 1e-6
HD = 128
SCALE = float(HD ** -0.5)


def build_C(s_len=S):
    p = P()
    SL = s_len
    NQT = SL // 128
    NJ = SL // 16
    NJT = (NJ + 127) // 128
    xT_d = p.dram("xT", [D, SL], F32, kind="ExternalInput")
    WF = p.dram("WF", [1, D, NSLOT * 128], F32, kind="ExternalInput")
    WT = p.dram("WT", [D, 640], F32, kind="ExternalInput")
    gmix = p.dram("gmix", [1, D], F32, kind="ExternalInput")
    gateb = p.dram("gateb", [1, 24], F32, kind="ExternalInput")
    peT = p.dram("peT", [2, 128, 32], F32, kind="ExternalInput")
    w1 = p.dram("w1", [2, 32, 128, 128], F32, kind="ExternalInput")
    w2 = p.dram("w2", [2, 128, 128], F32, kind="ExternalInput")
    ropeC = p.dram("ropeC", [32, SL], F32, kind="ExternalInput")
    ropeS = p.dram("ropeS", [32, SL], F32, kind="ExternalInput")
    ov_d = p.dram("ov", [NJT * 128, 64], F32, kind="ExternalInput")
    cmask_d = p.dram("cmask", [NQT, NJT, 128, 128], F32, kind="ExternalInput")
    vadd_d = p.dram("vadd", [NQT, 128, 64], F32, kind="ExternalInput")
    forced_d = p.dram("forced", [NQT, 128, 64], F32, kind="ExternalInput")
    expall_d = p.dram("expall", [64, SL], F32, kind="ExternalInput")
    causal_d = p.dram("causal", [128, 128], F32, kind="ExternalInput")
    far_d = p.dram("farm", [128, 128], F32, kind="ExternalInput")
    ident_d = p.dram("ident_in", [128, 128], F32, kind="ExternalInput")
    o_d = p.dram("o", [SL, 1024], F32, kind="ExternalOutput")
    scF = p.dram("scF", [NSLOT * 128, SL], F32, nsub=NSLOT)
    scT = p.dram("scT", [SL, 640], F32)
    scQr = p.dram("scQr", [8 * 32, SL], F32)

    def sF(slot, r0, r1, c0=0, c1=None):
        c1 = SL if c1 is None else c1
        return V(scF.t[slot * 128 + r0:slot * 128 + r1, c0:c1], [("scF", slot)])

    banks = Banks(p)
    ident = p.sb("ident", [128, 128], F32)
    p.dma("sp", ident[:], ident_d[:])
    ones_f = p.sb("ones_f", [128, 128], F32)
    p.memset(ones_f[:], 1.0)
    eps_t = p.sb("eps_t", [128, 1], F32)
    p.memset(eps_t[:], EPS)
    g_sb = p.sb("g_sb", [128, 1, KC], F32)
    p.dma("sp", g_sb[:, 0, :], dv(gmix, gmix.t[0].rearrange("(k p) -> p k", p=128)), allow_slow_non_contiguous=True)

    with p.scope():
        xT = p.sb("xTs", [128, KC, TB], F32)
        xn = p.sb("xn", [128, KC, TB], BF16)
        rstd = p.sb("rstd", [128, TB], F32)
        sqring = Ring(p, "sq", [128, TB], F32, 2)
        wring = Ring(p, "wr_", [128, KC, 128], BF16, 4)
        wtring = Ring(p, "wt_", [128, KC, 512], BF16, 2)
        stage = Ring(p, "stg", [128, TB], F32, 4)
        for tb in range(SL // TB):
            t0 = tb * TB
            p.dma("sp", xT[:], dv(xT_d, xT_d.t[:].rearrange("(k p) t -> p k t", p=128)[:, :, t0:t0 + TB]))
            rmsnorm_F(p, banks, xT, TB, g_sb, 0, xn, ones_f, sqring, eps_t, rstd)

            def cons_f(cc, ps, t0=t0):
                st = stage.get()
                p.copy(st[:, :], ps[:, :], eng="act")
                p.dma("sp", sF(cc, 0, 128, t0, t0 + TB), st[:, :])
            linear_F(p, banks, wring, WF, 0, xn, TB, cons_f, ccs=range(NSLOT))
            for cg, (c0, cw) in enumerate(((0, 512), (512, 128))):
                wt = wtring.get()
                p.dma("pool", wt[:, :, 0:cw], dv(WT, WT.t[:].rearrange("(k p) c -> p k c", p=128)[:, :, c0:c0 + cw]))
                for tt in range(TB // 128):
                    ps = banks.get()
                    for k in range(KC):
                        p.mm(ps[:, 0:cw], xn[:, k, tt * 128:(tt + 1) * 128], wt[:, k, 0:cw], start=(k == 0), stop=(k == KC - 1))
                    st = stage.get()
                    p.copy(st[:, 0:cw], ps[:, 0:cw], eng="act")
                    p.dma("sp", dv(scT, scT.t[t0 + tt * 128:t0 + (tt + 1) * 128, c0:c0 + cw]), st[:, 0:cw])

    with p.scope():
        rc = p.sb("rc", [32, SL], F32)
        rs = p.sb("rs", [32, SL], F32)
        p.dma("sp", rc[:], ropeC[:])
        p.dma("sp", rs[:], ropeS[:])
        mring = Ring(p, "rp_m", [32, SL], F32, 2)
        sring = Ring(p, "rp_s", [32, SL], F32, 2)
        jobs = [(h, 8 + h, ("q", h)) for h in range(8)]
        for g in range(2):
            jobs.append((16 + 6 * g + 2, 16 + 6 * g + 4, ("k", 0)))
            jobs.append((16 + 6 * g + 3, 16 + 6 * g + 5, ("k", 0)))
        for (ms, ss, (kind, h)) in jobs:
            m = mring.get()
            s_ = sring.get()
            p.dma("sp", m[:], sF(ms, 0, 32))
            p.dma("sp", s_[:], sF(ss, 0, 32))
            p.tt(m[:], m[:], rc[:], ALU.mult)
            p.tt(s_[:], s_[:], rs[:], ALU.mult)
            p.tt(m[:], m[:], s_[:], ALU.add)
            if kind == "q":
                p.dma("sp", dv(scQr, scQr.t[h * 32:(h + 1) * 32, :]), m[:])
            else:
                p.dma("sp", sF(ms, 0, 32), m[:])

    for g in range(2):
        with p.scope():
            sl0 = 16 + 6 * g
            KcT = p.sb("KcT", [128, NJT * 128], F32)
            Vc = p.sb("Vc", [128, NJT, 193], F32)
            p.memset(KcT[:], 0.0)
            p.memset(Vc[:], 0.0)
            for jt in range(NJT):
                p.memset(Vc[:, jt, 128:129], 1.0)
                p.dma("sp", Vc[:, jt, 129:193], dv(ov_d, ov_d.t[jt * 128:(jt + 1) * 128, :]))
            with p.scope():
                src_t = p.sb("cmp_src", [128, SL + 16], F32)
                w1s = p.sb("cmp_w1", [128, 32, 128], F32)
                w2s = p.sb("cmp_w2", [128, 128], F32)
                pes = p.sb("cmp_pe", [128, 32], F32)
                hid = p.sb("cmp_hid", [128, NJT * 128], F32)
                t1 = p.sb("cmp_t1", [128, NJT * 128], F32)
                t2 = p.sb("cmp_t2", [128, NJT * 128], F32)
                bias = p.sb("cmp_bias", [128, 1], F32)
                NJR = NJ - 1
                for kv in range(2):
                    p.memset(src_t[:, SL:SL + 16], 0.0)
                    p.dma("sp", src_t[:, 0:SL], sF(sl0 + kv, 0, 128))
                    p.dma("sp", w1s[:], dv(w1, w1.t[kv].rearrange("q d e -> d q e")))
                    p.dma("sp", w2s[:], dv(w2, w2.t[kv]))
                    p.dma("sp", pes[:], dv(peT, peT.t[kv]))
                    pb_ = banks.get()
                    for q_ in range(32):
                        p.mm(pb_[:, 0:1], w1s[:, q_, :], pes[:, q_:q_ + 1], start=(q_ == 0), stop=(q_ == 31))
                    p.copy(bias[:, :], pb_[:, 0:1])
                    ph = banks.get()
                    s3 = V(src_t.t[:, 0:SL + 16].rearrange("p (j s) -> p j s", s=16), src_t[:].keys)
                    for q_ in range(32):
                        rhs = V(s3.ap[:, (q_ // 16):(q_ // 16) + NJR, q_ % 16], src_t[:].keys)
                        p.mm(ph[:, 0:NJR], w1s[:, q_, :], rhs, start=(q_ == 0), stop=(q_ == 31))
                    x_ = hid
                    p.ts(x_[:, 0:NJR], ph[:, 0:NJR], bias[:, 0:1], ALU.add)
                    p.tt(t1[:, 0:NJR], x_[:, 0:NJR], x_[:, 0:NJR], ALU.mult)
                    p.ts(t1[:, 0:NJR], t1[:, 0:NJR], 0.044715, ALU.mult, 1.0, ALU.add)
                    p.tt(t1[:, 0:NJR], t1[:, 0:NJR], x_[:, 0:NJR], ALU.mult)
                    p.act(t2[:, 0:NJR], t1[:, 0:NJR], AF.Tanh, scale=0.7978845608028654)
                    p.ts(t2[:, 0:NJR], t2[:, 0:NJR], 1.0, ALU.add, 0.5, ALU.mult)
                    p.tt(hid[:, 0:NJR], t2[:, 0:NJR], x_[:, 0:NJR], ALU.mult)
                    if kv == 0:
                        pk = banks.get()
                        p.mm(pk[:, 0:NJR], w2s[:, :], hid[:, 0:NJR])
                        p.copy(KcT[:, 0:NJR], pk[:, 0:NJR])
                    else:
                        for jt in range(NJT):
                            n = min(128, NJR - jt * 128)
                            pv = banks.get()
                            p.mm(pv[0:n, 0:128], hid[:, jt * 128:jt * 128 + n], w2s[:, :])
                            p.copy(Vc[0:n, jt, 0:128], pv[0:n, 0:128])
            ksT = p.sb("ksT", [128, SL], BF16)
            kwT = p.sb("kwT", [128, SL], BF16)
            p.dma("pool", ksT[:], sF(sl0 + 2, 0, 128))
            p.dma("pool", kwT[:], sF(sl0 + 3, 0, 128))
            vs = p.sb("vs", [128, NQT, 129], BF16)
            vw = p.sb("vw", [128, NQT, 129], BF16)
            p.memset(vs[:, :, 128:129], 1.0)
            p.memset(vw[:, :, 128:129], 1.0)
            p.dma("pool", vs[:, :, 0:128], dv(scT, scT.t[:, g * 128:(g + 1) * 128].rearrange("(k p) d -> p k d", p=128)))
            p.dma("pool", vw[:, :, 0:128], dv(scT, scT.t[:, 256 + g * 128:256 + (g + 1) * 128].rearrange("(k p) d -> p k d", p=128)))
            expall = p.sb("expall_s", [64, SL], F32)
            p.dma("sp", expall[:], expall_d[:])
            causal = p.sb("causal_s", [128, 128], F32)
            p.dma("sp", causal[:], causal_d[:])
            farm = p.sb("farm_s", [128, 128], F32)
            p.dma("sp", farm[:], far_d[:])
            gbb = p.sb("gbb", [128, 24], F32)
            p.dma("sp", gbb[:], dv(gateb, gateb.t[0].partition_broadcast(128)))
            eall = p.sb("eall", [128, NQT, 512], BF16, nsub=NQT)
            ewin = p.sb("ewin", [128, 5, 512], BF16, nsub=5)
            qn_r = Ring(p, "qn", [128, 4, 128], F32, 2)
            qr_r = Ring(p, "qr", [128, 4, 128], BF16, 2)
            gl_r = Ring(p, "gl", [128, 24], F32, 2)
            cm_r = Ring(p, "cm", [128, NJT, 128], F32, 2)
            va_r = Ring(p, "va", [128, 64], F32, 2)
            fo_r = Ring(p, "fo", [128, 64], F32, 2)
            ec_r = Ring(p, "ec", [128, NJT, 512], F32, 2)
            et_r = Ring(p, "et", [128, 512], F32, 3)
            ms_r = Ring(p, "msk", [128, 128], F32, 2)
            sm_r = Ring(p, "smc", [128, 320], F32, 2)
            selT_r = Ring(p, "selT", [64, 128], F32, 2)
            ot_r = Ring(p, "ot", [128, 512], F32, 2)
            u32 = mybir.dt.uint32
            for i in range(NQT):
                qs = slice(i * 128, (i + 1) * 128)
                qn = qn_r.get()
                qr = qr_r.get()
                for r in range(4):
                    h = 4 * g + r
                    p.dma("sp", qn[:, r, :], sF(h, 0, 128, i * 128, (i + 1) * 128))
                    p.dma("pool", qr[32:128, r, :], sF(h, 32, 128, i * 128, (i + 1) * 128))
                    p.dma("pool", qr[0:32, r, :], dv(scQr, scQr.t[h * 32:(h + 1) * 32, qs]))
                gl = gl_r.get()
                p.dma("sp", gl[:], dv(scT, scT.t[qs, 512:536]))
                p.tt(gl[:], gl[:], gbb[:], ALU.add)
                p.act(gl[:], gl[:], AF.Sigmoid)
                cm = cm_r.get()
                p.dma("sp", cm[:], dv(cmask_d, cmask_d.t[i].rearrange("j p q -> p j q")))
                va = va_r.get()
                p.dma("sp", va[:], dv(vadd_d, vadd_d.t[i]))
                fo = fo_r.get()
                p.dma("sp", fo[:], dv(forced_d, forced_d.t[i]))
                ot = ot_r.get()
                sm = sm_r.get()
                njt = min(NJT, (8 * i + 6) // 128 + 1)
                qn2 = V(qn.t[:].rearrange("p r q -> p (r q)"), qn[:].keys)
                qr2 = V(qr.t[:].rearrange("p r q -> p (r q)"), qr[:].keys)
                ec = ec_r.get()
                for jt in range(njt):
                    ps = banks.get()
                    p.mm(ps[:, :], KcT[:, jt * 128:(jt + 1) * 128], qn2)
                    p.act(ec[:, jt, :], ps[:, :], AF.Exp, scale=SCALE)
                    cmb = V(cm.t[:, jt, :].unsqueeze(1).broadcast_to([128, 4, 128]), cm[:].keys)
                    e3 = V(ec.t[:, jt, :].rearrange("p (r q) -> p r q", r=4), ec[:].keys)
                    p.tt(e3, e3, cmb, ALU.mult)
                for r in range(4):
                    h = 4 * g + r
                    po = banks.get()
                    for jt in range(njt):
                        p.mm(po[:, 0:193], ec[:, jt, r * 128:(r + 1) * 128], Vc[:, jt, :], start=(jt == 0), stop=(jt == njt - 1))
                    p.ts(sm[:, 272:273], po[:, 128:129], 1e-30, ALU.max)
                    p.recip(sm[:, 273:274], sm[:, 272:273])
                    if r == 0:
                        p.ts(sm[:, 0:64], po[:, 129:193], sm[:, 273:274], ALU.mult)
                    else:
                        p.stt(sm[:, 0:64], po[:, 129:193], sm[:, 273:274], sm[:, 0:64], ALU.mult, ALU.add)
                    p.tt(sm[:, 274:275], sm[:, 273:274], gl[:, h:h + 1], ALU.mult)
                    p.ts(ot[:, r * 128:(r + 1) * 128], po[:, 0:128], sm[:, 274:275], ALU.mult)
                p.tt(sm[:, 64:128], sm[:, 0:64], va[:], ALU.add)
                p.op("dve", lambda e: e.max(sm.t[:, 192:200], sm.t[:, 64:128]), [sm[:]], [sm[:]])
                p.op("dve", lambda e: e.match_replace(sm.t[:, 128:192], sm.t[:, 192:200], sm.t[:, 64:128], -1e30), [sm[:]], [sm[:]])
                p.op("dve", lambda e: e.max(sm.t[:, 200:208], sm.t[:, 128:192]), [sm[:]], [sm[:]])
                p.ts(sm[:, 208:272], sm[:, 64:128], sm[:, 204:205], ALU.is_ge)
                p.ts(sm[:, 128:192], sm[:, 64:128], -1e29, ALU.is_gt)
                p.tt(sm[:, 208:272], sm[:, 208:272], sm[:, 128:192], ALU.mult)
                p.tt(sm[:, 208:272], sm[:, 208:272], fo[:], ALU.add)
                pt = banks.get()
                p.tr(pt[0:64, 0:128], sm[:, 208:272], ident[:, :])
                selT = selT_r.get()
                p.copy(selT[:, :], pt[0:64, 0:128])
                for kt in range(i + 1):
                    ks_ = slice(kt * 128, (kt + 1) * 128)
                    ps = banks.get()
                    p.mm(ps[:, :], ksT[:, ks_], qr2)
                    et = et_r.get()
                    p.act(et[:, :], ps[:, :], AF.Exp, scale=SCALE)
                    pm_ = banks.get()
                    p.mm(pm_[:, 0:128], expall[:, ks_], selT[:, :])
                    if kt == i:
                        mk = ms_r.get()
                        p.tt(mk[:, :], pm_[:, 0:128], causal[:, :], ALU.mult)
                        mb = V(mk.t[:, :].unsqueeze(1).broadcast_to([128, 4, 128]), mk[:].keys)
                    else:
                        mb = V(pm_.t[:, 0:128].unsqueeze(1).broadcast_to([128, 4, 128]), pm_[:].keys)
                    p.tt(V(eall.t[:, kt, :].rearrange("p (r q) -> p r q", r=4), eall.k(kt)[:, kt, :].keys),
                         V(et.t[:, :].rearrange("p (r q) -> p r q", r=4), et[:].keys), mb, ALU.mult)
                for r in range(4):
                    h = 4 * g + r
                    po = banks.get()
                    for kt in range(i + 1):
                        p.mm(po[:, 0:129], eall.k(kt)[:, kt, r * 128:(r + 1) * 128], vs[:, kt, :], start=(kt == 0), stop=(kt == i))
                    p.ts(sm[:, 275:276], po[:, 128:129], 1e-30, ALU.max)
                    p.recip(sm[:, 276:277], sm[:, 275:276])
                    p.tt(sm[:, 277:278], sm[:, 276:277], gl[:, 8 + h:9 + h], ALU.mult)
                    p.stt(ot[:, r * 128:(r + 1) * 128], po[:, 0:128], sm[:, 277:278], ot[:, r * 128:(r + 1) * 128], ALU.mult, ALU.add)
                kts = list(range(max(0, i - 4), i + 1))
                for n_, kt in enumerate(kts):
                    ks_ = slice(kt * 128, (kt + 1) * 128)
                    ps = banks.get()
                    p.mm(ps[:, :], kwT[:, ks_], qr2)
                    dst = V(ewin.t[:, n_, :], ewin.k(n_)[:, n_, :].keys)
                    if kt == i or kt == i - 4:
                        et = et_r.get()
                        p.act(et[:, :], ps[:, :], AF.Exp, scale=SCALE)
                        mk = causal if kt == i else farm
                        mb = V(mk.t[:, :].unsqueeze(1).broadcast_to([128, 4, 128]), mk[:].keys)
                        p.tt(V(ewin.t[:, n_, :].rearrange("p (r q) -> p r q", r=4), ewin.k(n_)[:, n_, :].keys),
                             V(et.t[:, :].rearrange("p (r q) -> p r q", r=4), et[:].keys), mb, ALU.mult)
                    else:
                        p.act(dst, ps[:, :], AF.Exp, scale=SCALE)
                for r in range(4):
                    h = 4 * g + r
                    po = banks.get()
                    for n_, kt in enumerate(kts):
                        p.mm(po[:, 0:129], ewin.k(n_)[:, n_, r * 128:(r + 1) * 128], vw[:, kt, :], start=(n_ == 0), stop=(n_ == len(kts) - 1))
                    p.ts(sm[:, 278:279], po[:, 128:129], 1e-30, ALU.max)
                    p.recip(sm[:, 279:280], sm[:, 278:279])
                    p.tt(sm[:, 280:281], sm[:, 279:280], gl[:, 16 + h:17 + h], ALU.mult)
                    p.stt(ot[:, r * 128:(r + 1) * 128], po[:, 0:128], sm[:, 280:281], ot[:, r * 128:(r + 1) * 128], ALU.mult, ALU.add)
                p.dma("sp", dv(o_d, o_d.t[qs, g * 512:(g + 1) * 512]), ot[:, :], is_output=True)
    p.finish()
    return p


def prep_A(inp, b, hh, SL):
    w_in = inp["ab_w_in"][0]
    ML = 3080
    def mlc(a, n): return list(range(a, a + n))
    cols = []
    pad = lambda lst, n: lst + [-1] * (n - len(lst))
    for h in (2 * hh, 2 * hh + 1): cols += mlc(h * 128, 128)
    for h in (2 * hh, 2 * hh + 1): cols += mlc(512 + h * 128, 128)
    g0 = 1024 + 1024 + 1024
    cols += pad([g0 + 2 * hh, g0 + 2 * hh + 1, g0 + 4 + 2 * hh, g0 + 4 + 2 * hh + 1], 128)
    for part in range(3): cols += mlc(ML + part * 1024 + hh * 512, 512)
    cols += pad(mlc(ML + 3072, 64), 128); cols += pad(mlc(ML + 3072 + 64, 64), 128); cols += mlc(ML + 3072 + 128, 128)
    cols = np.array(cols)
    WF = np.where(cols[None, :] >= 0, w_in[:, np.maximum(cols, 0)], 0.0).astype(np.float32)
    WT = np.concatenate([w_in[:, 1024 + 2 * hh * 256:1024 + (2 * hh + 2) * 256], w_in[:, 2048 + 2 * hh * 256:2048 + (2 * hh + 2) * 256]], axis=1)
    cw = inp["ml_conv_w"][0]; cb = inp["ml_conv_b"][0]
    qkcols = [(2 * hh) * 128, (2 * hh + 1) * 128, 512 + (2 * hh) * 128, 512 + (2 * hh + 1) * 128]
    convw = np.stack([cw[:, c0:c0 + 128].T for c0 in qkcols], axis=1)
    convb = np.stack([cb[c0:c0 + 128] for c0 in qkcols], axis=1)
    gb = inp["ml_gate_b"][0]
    gateb = np.array([[gb[2 * hh], gb[2 * hh + 1], gb[4 + 2 * hh], gb[4 + 2 * hh + 1]]], np.float32)
    mu = inp["rw_mu"][0]
    def hv(v): return v[hh * 512:(hh + 1) * 512].reshape(8, 64).T
    vecs = [hv(mu[0:1024]), hv(mu[1024:2048]), hv(mu[2048:3072])] + [hv(inp[n][0]) for n in ["rw_w0", "rw_a0", "rw_k_k", "rw_k_a", "rw_r_k", "rw_ln_w", "rw_ln_b"]]
    rwvec = np.stack(vecs, axis=1).astype(np.float32)
    mulora = np.zeros((128, 3), np.float32)
    mulora[:64, 0] = mu[3072:3136]; mulora[:64, 1] = mu[3136:3200]; mulora[:, 2] = mu[3200:3328]
    sl = slice(hh * 512, (hh + 1) * 512)
    tri = np.arange(128)
    mlmask = np.where(tri[:, None] <= tri[None, :], 0.0, -1e30).astype(np.float32)
    t64 = np.arange(64)
    strict = (t64[:, None] < t64[None, :]).astype(np.float32); incl = (t64[:, None] <= t64[None, :]).astype(np.float32)
    lower = (t64[:, None] > t64[None, :]).astype(np.float32)
    rwmask = np.stack([strict, strict, incl, incl, lower], axis=1)
    return dict(xT=np.ascontiguousarray(inp["x"][b, :SL].T), WF=np.ascontiguousarray(WF[None]), WT=np.ascontiguousarray(WT),
                gmix=inp["norm_mix"][0:1], convw=np.ascontiguousarray(convw), convb=np.ascontiguousarray(convb), gateb=gateb,
                rwvec=np.ascontiguousarray(rwvec), mulora=mulora, wup=np.ascontiguousarray(inp["rw_w_up"][0][:, sl]),
                aup=np.ascontiguousarray(inp["rw_a_up"][0][:, sl]), gup=np.ascontiguousarray(inp["rw_g_up"][0][:, sl]),
                ident_in=np.eye(128, dtype=np.float32), mlmask=mlmask, rwmask=np.ascontiguousarray(rwmask))


def prep_C(xfull, nsa_w_in, nsa_gate_b, cmp_pos, cmp_w1, cmp_w2, gmix, b, hh, SL):
    w_in = nsa_w_in
    q0, kc0, vc0, ks0, vs0, kw0, vw0, g0 = 0, 2048, 2560, 3072, 3584, 4096, 4608, 5120
    perm = np.array(list(range(16, 32)) + list(range(0, 16)))
    cols = []
    pad = lambda lst, n: list(lst) + [-1] * (n - len(lst))
    for h in range(8): cols += list(range((8 * hh + h) * 128, (8 * hh + h + 1) * 128))
    for h in range(8): cols += pad((8 * hh + h) * 128 + perm, 128)
    for g in range(2):
        G = 2 * hh + g
        cols += list(range(kc0 + G * 128, kc0 + (G + 1) * 128)); cols += list(range(vc0 + G * 128, vc0 + (G + 1) * 128))
        cols += list(range(ks0 + G * 128, ks0 + (G + 1) * 128)); cols += list(range(kw0 + G * 128, kw0 + (G + 1) * 128))
        cols += pad(ks0 + G * 128 + perm, 128); cols += pad(kw0 + G * 128 + perm, 128)
    cols = np.array(cols)
    WF = np.where(cols[None, :] >= 0, w_in[:, np.maximum(cols, 0)], 0.0).astype(np.float32)
    gcols = [g0 + br * 16 + 8 * hh + h for br in range(3) for h in range(8)]
    tcols = []
    for base in (vs0, vw0):
        for g in range(2):
            G = 2 * hh + g
            tcols += list(range(base + G * 128, base + (G + 1) * 128))
    tcols += pad(gcols, 128)
    tcols = np.array(tcols)
    WT = np.where(tcols[None, :] >= 0, w_in[:, np.maximum(tcols, 0)], 0.0).astype(np.float32)
    gateb = nsa_gate_b[[br * 16 + 8 * hh + h for br in range(3) for h in range(8)]][None].astype(np.float32)
    inv = 1.0 / (500000.0 ** (np.arange(16, dtype=np.float32) / 16))
    ang = np.arange(SL, dtype=np.float32)[None, :] * inv[:, None].astype(np.float32)
    cos = np.cos(ang).astype(np.float32); sin = np.sin(ang).astype(np.float32)
    ropeC = np.concatenate([cos, cos], 0); ropeS = np.concatenate([-sin, sin], 0)
    NQT = SL // 128; NJ = SL // 16; NJT = (NJ + 127) // 128
    n_cmp = (SL - 32) // 16 + 1; n_sel = SL // 64
    c0 = np.arange(NJT * 128)[:, None] * 16; s0 = np.arange(64)[None, :] * 64
    ov = np.clip(np.minimum(c0 + 32, s0 + 64) - np.maximum(c0, s0), 0, None) / 32.0
    ov[n_cmp:] = 0; ov[:, n_sel:] = 0
    j = np.arange(NJT * 128); qt = np.arange(SL)
    cm = ((16 * j[:, None] + 31 <= qt[None, :]) & (j[:, None] < n_cmp)).astype(np.float32)
    cmask = cm.reshape(NJT, 128, NQT, 128).transpose(2, 0, 1, 3)
    cur = qt // 64; s = np.arange(64)
    validnf = (s[None, :] >= 1) & (s[None, :] <= cur[:, None] - 2)
    vadd = np.where(validnf, 0.0, -1e30).astype(np.float32).reshape(NQT, 128, 64)
    forced = ((s[None, :] == 0) | (s[None, :] == cur[:, None]) | (s[None, :] == cur[:, None] - 1)).astype(np.float32).reshape(NQT, 128, 64)
    expall = (np.arange(SL)[None, :] // 64 == s[:, None]).astype(np.float32)
    t = np.arange(128)
    causal = (t[:, None] <= t[None, :]).astype(np.float32); farm = (t[:, None] > t[None, :]).astype(np.float32)
    return dict(xT=np.ascontiguousarray(xfull.T), WF=np.ascontiguousarray(WF[None]), WT=np.ascontiguousarray(WT), gmix=gmix[None].astype(np.float32),
                gateb=gateb, peT=np.ascontiguousarray(cmp_pos.transpose(0, 2, 1)), w1=np.ascontiguousarray(cmp_w1), w2=np.ascontiguousarray(cmp_w2),
                ropeC=np.ascontiguousarray(ropeC), ropeS=np.ascontiguousarray(ropeS), ov=np.ascontiguousarray(ov.astype(np.float32)),
                cmask=np.ascontiguousarray(cmask), vadd=vadd, forced=forced, expall=expall, causal=causal, farm=farm, ident_in=np.eye(128, dtype=np.float32))


def _run_B(prog, inp, l, xfull, ycat, w_out):
    mem = inp["mem"]
    gains = np.stack([inp["norm_cross"][l], inp["norm_mem"][l], inp["norm_ffn"][l], inp["norm_final"]]).astype(np.float32)
    wr = np.ascontiguousarray(np.concatenate([inp["moe_wg"][l], inp["moe_we"][l]], axis=1).astype(np.float32))
    br = np.concatenate([inp["moe_bg"][l], inp["moe_be"][l]]).astype(np.float32)
    shared = dict(w_out=np.ascontiguousarray(w_out), ca_wq=inp["ca_wq"][l:l + 1], ca_wk=inp["ca_wk"][l:l + 1],
                  ca_wv=inp["ca_wv"][l:l + 1], ca_wo=inp["ca_wo"][l:l + 1], gains=gains, wr=wr, br=br,
                  wgate=inp["moe_w_gate"][l], wup=inp["moe_w_up"][l], wdown=inp["moe_w_down"][l],
                  ident_in=np.eye(128, dtype=np.float32))
    in_maps = []
    for c in range(8):
        b, h = c // 2, c % 2
        sl = slice(h * 2048, (h + 1) * 2048)
        m = dict(shared)
        m["xT"] = np.ascontiguousarray(xfull[b, sl].T)
        m["ycT"] = np.ascontiguousarray(ycat[b, sl].T)
        m["memT"] = np.ascontiguousarray(mem[b].T)
        in_maps.append(m)
    res = run_bass_kernel_spmd(prog.nc, in_maps, core_ids=list(range(8)))
    out = np.zeros((4, 4096, 2048), np.float32)
    for c in range(8):
        b, h = c // 2, c % 2
        out[b, h * 2048:(h + 1) * 2048] = res.results[c]["outT"].T
    return out


def kernel(**inputs):
    inp = {k: np.ascontiguousarray(np.asarray(v)) for k, v in inputs.items()}
    x = inp["x"].astype(np.float32)
    SL = 4096
    pA = build_A()
    resA = run_bass_kernel_spmd(pA.nc, [prep_A(inp, c // 2, c % 2, SL) for c in range(8)], core_ids=list(range(8)))
    ycat = np.zeros((4, SL, 2048), np.float32)
    for c in range(8):
        b, hh = c // 2, c % 2
        ycat[b, :, hh * 512:(hh + 1) * 512] = resA.results[c]["ym"]
        ycat[b, :, 1024 + hh * 512:1024 + (hh + 1) * 512] = resA.results[c]["yrT"].T
    del resA
    pB0 = build_B(final_norm=False)
    x1 = _run_B(pB0, inp, 0, x, ycat, inp["ab_w_out"][0:1])
    pC = build_C()
    resC = run_bass_kernel_spmd(pC.nc, [prep_C(x1[c // 2], inp["nsa_w_in"][0], inp["nsa_gate_b"][0], inp["cmp_pos"][0],
                                               inp["cmp_w1"][0], inp["cmp_w2"][0], inp["norm_mix"][1], c // 2, c % 2, SL)
                                        for c in range(8)], core_ids=list(range(8)))
    ocat = np.zeros((4, SL, 2048), np.float32)
    for c in range(8):
        b, hh = c // 2, c % 2
        ocat[b, :, hh * 1024:(hh + 1) * 1024] = resC.results[c]["o"]
    del resC
    pB1 = build_B(final_norm=True)
    out = _run_B(pB1, inp, 1, x1, ocat, inp["nsa_w_out"][0:1])
    return out.astype(np.float32)
```
